# Optimizing a Trainium2 kernel written in Bass

```python
import jax, jax.numpy as jnp
from jax import lax
import numpy as np

D_MODEL = 2048
BATCH = 4
SEQ = 2048
DEPTH = 1

HEAD_DIM = 128
N_Q_HEADS = 8
N_KV_GROUPS = 2
HEADS_PER_GROUP = N_Q_HEADS // N_KV_GROUPS
NSA_WIDTH = N_Q_HEADS * HEAD_DIM
KV_WIDTH = N_KV_GROUPS * HEAD_DIM
N_BRANCH = 3
N_GATES = N_BRANCH * N_Q_HEADS
CONV_WIDTH = D_MODEL - NSA_WIDTH
MIX_WIDTH = NSA_WIDTH + CONV_WIDTH
IN_SPLITS = (NSA_WIDTH,) + (KV_WIDTH,) * 6 + (N_GATES, 2 * CONV_WIDTH)
IN_WIDTH = sum(IN_SPLITS)

CMP_BLOCK = 32
CMP_STRIDE = 16
SEL_BLOCK = 64
SEL_TOPK = 16
SEL_QCHUNK = 64
WINDOW = 512
WIN_QBLOCK = 128
ROPE_THETA = 10000.0

CONV_KERNEL = 31

N_EXPERTS = 256
TOP_K = 8
N_EXPERT_GROUPS = 8
TOPK_GROUPS = 4
EXPERT_HIDDEN = 512
SHARED_HIDDEN = 512
ROUTE_SCALE = 2.5
DISPATCH_BLOCK = 128

EPS = 1e-6
NEG_INF = -1e30
FORCE = 1e30

kernel_name = 'hymba_nsa_conformer_moe_block'


def rms_norm(x, g):
    xf = x.astype(jnp.float32)
    y = xf * lax.rsqrt(jnp.mean(xf * xf, axis=-1, keepdims=True) + EPS)
    return (y * g.astype(jnp.float32)).astype(x.dtype)


def rope(t, pos):
    half = t.shape[-1] // 2
    inv = ROPE_THETA ** (-jnp.arange(half, dtype=jnp.float32) / half)
    ang = pos.astype(jnp.float32)[..., None] * inv
    cos = jnp.cos(ang)[:, :, None, :]
    sin = jnp.sin(ang)[:, :, None, :]
    tf = t.astype(jnp.float32)
    t1, t2 = tf[..., :half], tf[..., half:]
    return jnp.concatenate([t1 * cos - t2 * sin, t1 * sin + t2 * cos], axis=-1).astype(t.dtype)


def compress_kv(kv, pe, w1, w2):
    B, S, G, d = kv.shape
    n_sub = CMP_BLOCK // CMP_STRIDE
    chunks = kv.reshape(B, S // CMP_STRIDE, CMP_STRIDE, G, d)
    nc = S // CMP_STRIDE - n_sub + 1
    blocks = jnp.concatenate([chunks[:, j:j + nc] for j in range(n_sub)], axis=2)
    blocks = blocks + pe[None, None, :, None, :]
    flat = blocks.transpose(0, 1, 3, 2, 4).reshape(B, nc, G, CMP_BLOCK * d)
    return jax.nn.silu(flat @ w1) @ w2


def nsa_attention(q, k_cmp, v_cmp, k_sel, v_sel, k_win, v_win, gates, positions,
                  cmp_k_pe, cmp_k_w1, cmp_k_w2, cmp_v_pe, cmp_v_w1, cmp_v_w2):
    B, S = q.shape[:2]
    G, HPG, d = N_KV_GROUPS, HEADS_PER_GROUP, HEAD_DIM
    scale = d ** -0.5
    t = jnp.arange(S)
    q = rope(q, positions)
    qg = q.reshape(B, S, G, HPG, d).transpose(0, 2, 3, 1, 4)

    k_c = rope(compress_kv(k_cmp, cmp_k_pe, cmp_k_w1, cmp_k_w2),
               positions[:, CMP_BLOCK - 1::CMP_STRIDE])
    v_c = compress_kv(v_cmp, cmp_v_pe, cmp_v_w1, cmp_v_w2)
    nc = k_c.shape[1]
    blk_end = jnp.arange(nc) * CMP_STRIDE + CMP_BLOCK - 1
    valid_c = blk_end[None, :] <= t[:, None]
    s_c = jnp.einsum('bghtd,bcgd->bghtc', qg, k_c).astype(jnp.float32) * scale
    s_c = jnp.where(valid_c, s_c, NEG_INF)
    any_c = (t >= CMP_BLOCK - 1).astype(jnp.float32)
    p_c = jax.nn.softmax(s_c, axis=-1) * any_c[:, None]
    o_c = jnp.einsum('bghtc,bcgd->bghtd', p_c.astype(v_c.dtype), v_c)

    n_sel = S // SEL_BLOCK
    cs = np.arange(nc) * CMP_STRIDE
    js = np.arange(n_sel) * SEL_BLOCK
    overlap = ((cs[:, None] < js[None, :] + SEL_BLOCK) &
               (cs[:, None] + CMP_BLOCK > js[None, :])).astype(np.float32)
    imp = jnp.einsum('bgtc,cj->bgtj', p_c.sum(axis=2), jnp.asarray(overlap))
    jb = jnp.arange(n_sel)
    cur = (t // SEL_BLOCK)[:, None]
    forced = (jb[None, :] == 0) | (jb[None, :] == cur) | (jb[None, :] == cur - 1)
    causal_blk = jb[None, :] * SEL_BLOCK <= t[:, None]
    imp = jnp.where(forced, FORCE, jnp.where(causal_blk, imp, NEG_INF))
    k_top = min(SEL_TOPK, n_sel)
    _, sel_idx = lax.top_k(imp, k_top)

    ks_r = rope(k_sel, positions)
    Kb = ks_r.reshape(B, n_sel, SEL_BLOCK, G, d).transpose(0, 3, 1, 2, 4)
    Vb = v_sel.reshape(B, n_sel, SEL_BLOCK, G, d).transpose(0, 3, 1, 2, 4)
    n_ch = S // SEL_QCHUNK
    q_ch = qg.reshape(B, G, HPG, n_ch, SEL_QCHUNK, d).transpose(3, 0, 1, 2, 4, 5)
    idx_ch = sel_idx.reshape(B, G, n_ch, SEL_QCHUNK, k_top).transpose(2, 0, 1, 3, 4)
    t_ch = t.reshape(n_ch, SEL_QCHUNK)
    bi = jnp.arange(B)[:, None, None, None]
    gi = jnp.arange(G)[None, :, None, None]
    offs = jnp.arange(SEL_BLOCK)

    def sel_block(args):
        qc, ic, tc = args
        kg = Kb[bi, gi, ic]
        vg = Vb[bi, gi, ic]
        s = jnp.einsum('bghtd,bgtkld->bghtkl', qc, kg).astype(jnp.float32) * scale
        kpos = ic[..., None] * SEL_BLOCK + offs
        m = kpos <= tc[None, None, :, None, None]
        s = jnp.where(m[:, :, None], s, NEG_INF)
        p = jax.nn.softmax(s.reshape(s.shape[:4] + (-1,)), axis=-1).reshape(s.shape)
        return jnp.einsum('bghtkl,bgtkld->bghtd', p.astype(vg.dtype), vg)

    o_s = lax.map(sel_block, (q_ch, idx_ch, t_ch))
    o_s = o_s.transpose(1, 2, 3, 0, 4, 5).reshape(B, G, HPG, S, d)

    nqb = S // WIN_QBLOCK
    span = WINDOW + WIN_QBLOCK
    kpad = jnp.pad(rope(k_win, positions), ((0, 0), (WINDOW, 0), (0, 0), (0, 0)))
    vpad = jnp.pad(v_win, ((0, 0), (WINDOW, 0), (0, 0), (0, 0)))
    win_idx = (jnp.arange(nqb) * WIN_QBLOCK)[:, None] + jnp.arange(span)[None, :]
    kw = kpad[:, win_idx]
    vw = vpad[:, win_idx]
    qw = qg.reshape(B, G, HPG, nqb, WIN_QBLOCK, d)
    s_w = jnp.einsum('bghnid,bnjgd->bghnij', qw, kw).astype(jnp.float32) * scale
    tq = t.reshape(nqb, WIN_QBLOCK)[:, :, None]
    kp = (win_idx - WINDOW)[:, None, :]
    m_w = (kp >= 0) & (kp <= tq) & (tq - kp < WINDOW)
    p_w = jax.nn.softmax(jnp.where(m_w, s_w, NEG_INF), axis=-1)
    o_w = jnp.einsum('bghnij,bnjgd->bghnid', p_w.astype(vw.dtype), vw).reshape(B, G, HPG, S, d)

    g = jax.nn.sigmoid(gates.astype(jnp.float32)).astype(q.dtype)
    g = g.reshape(B, S, N_BRANCH, G, HPG).transpose(2, 0, 3, 4, 1)[..., None]
    o = g[0] * o_c + g[1] * o_s + g[2] * o_w
    return o.transpose(0, 3, 1, 2, 4).reshape(B, S, NSA_WIDTH)


def conformer_conv(u, conv_w, conv_b, ln_g, ln_b):
    a, b = jnp.split(u, 2, axis=-1)
    y = a * jax.nn.sigmoid(b)
    y = lax.conv_general_dilated(y, conv_w, window_strides=(1,),
                                 padding=[(CONV_KERNEL - 1, 0)],
                                 dimension_numbers=('NWC', 'WIO', 'NWC'),
                                 feature_group_count=CONV_WIDTH) + conv_b
    yf = y.astype(jnp.float32)
    mu = jnp.mean(yf, axis=-1, keepdims=True)
    var = jnp.mean(jnp.square(yf - mu), axis=-1, keepdims=True)
    yn = (yf - mu) * lax.rsqrt(var + EPS) * ln_g.astype(jnp.float32) + ln_b.astype(jnp.float32)
    return jax.nn.silu(yn).astype(u.dtype)


def hybrid_mixer(h, positions, w_in, cmp_k_pe, cmp_k_w1, cmp_k_w2, cmp_v_pe, cmp_v_w1,
                 cmp_v_w2, conv_w, conv_b, conv_ln_g, conv_ln_b, w_out):
    B, S, _ = h.shape
    proj = h @ w_in
    split_pts = np.cumsum(IN_SPLITS)[:-1].tolist()
    q, kc, vc, ksl, vsl, kwn, vwn, gates, glu_in = jnp.split(proj, split_pts, axis=-1)
    qh = q.reshape(B, S, N_Q_HEADS, HEAD_DIM)
    kvh = lambda z: z.reshape(B, S, N_KV_GROUPS, HEAD_DIM)
    o_attn = nsa_attention(qh, kvh(kc), kvh(vc), kvh(ksl), kvh(vsl), kvh(kwn), kvh(vwn),
                           gates, positions, cmp_k_pe, cmp_k_w1, cmp_k_w2,
                           cmp_v_pe, cmp_v_w1, cmp_v_w2)
    o_conv = conformer_conv(glu_in, conv_w, conv_b, conv_ln_g, conv_ln_b)
    return jnp.concatenate([o_attn, o_conv], axis=-1) @ w_out


def route(h2, w_router, router_bias):
    T = h2.shape[0]
    s = jax.nn.sigmoid((h2 @ w_router).astype(jnp.float32))
    sb = s + router_bias.astype(jnp.float32)
    grp = sb.reshape(T, N_EXPERT_GROUPS, N_EXPERTS // N_EXPERT_GROUPS)
    grp_score = lax.top_k(grp, 2)[0].sum(-1)
    _, gidx = lax.top_k(grp_score, TOPK_GROUPS)
    gmask = jax.nn.one_hot(gidx, N_EXPERT_GROUPS).sum(-2) > 0
    emask = jnp.repeat(gmask, N_EXPERTS // N_EXPERT_GROUPS, axis=-1)
    _, top_idx = lax.top_k(jnp.where(emask, sb, NEG_INF), TOP_K)
    top_s = jnp.take_along_axis(s, top_idx, axis=-1)
    top_w = top_s / jnp.sum(top_s, axis=-1, keepdims=True) * ROUTE_SCALE
    return top_idx, top_w


def routed_experts(h2, top_idx, top_w, w_gate, w_up, w_down):
    T, D = h2.shape
    E = w_gate.shape[0]
    A = T * TOP_K
    e_flat = top_idx.reshape(A)
    tok_flat = jnp.arange(A, dtype=jnp.int32) // TOP_K
    w_flat = top_w.reshape(A)
    order = jnp.argsort(e_flat)
    e_sorted = e_flat[order]
    counts = jnp.bincount(e_flat, length=E)
    starts = jnp.cumsum(counts) - counts
    padded = (counts + DISPATCH_BLOCK - 1) // DISPATCH_BLOCK * DISPATCH_BLOCK
    pad_ends = jnp.cumsum(padded)
    pad_starts = pad_ends - padded
    dest = pad_starts[e_sorted] + (jnp.arange(A) - starts[e_sorted])
    n_blocks = -(-(A + E * (DISPATCH_BLOCK - 1)) // DISPATCH_BLOCK)
    P = n_blocks * DISPATCH_BLOCK
    row_tok = jnp.full((P,), T, jnp.int32).at[dest].set(tok_flat[order])
    row_w = jnp.zeros((P,), h2.dtype).at[dest].set(w_flat[order])
    block_e = jnp.minimum(jnp.searchsorted(pad_ends, jnp.arange(n_blocks) * DISPATCH_BLOCK,
                                           side='right'), E - 1)
    h_pad = jnp.concatenate([h2, jnp.zeros((1, D), h2.dtype)], axis=0)

    def block_fn(args):
        toks, wts, e = args
        xb = h_pad[toks]
        y = (jax.nn.silu(xb @ w_gate[e]) * (xb @ w_up[e])) @ w_down[e]
        return y * wts[:, None]

    y = lax.map(block_fn, (row_tok.reshape(n_blocks, DISPATCH_BLOCK),
                           row_w.reshape(n_blocks, DISPATCH_BLOCK), block_e))
    return jax.ops.segment_sum(y.reshape(P, D), row_tok, num_segments=T + 1)[:T]


def moe_ffn(h, w_router, router_bias, w_exp_gate, w_exp_up, w_exp_down,
            w_sh_gate, w_sh_up, w_sh_down):
    B, S, D = h.shape
    h2 = h.reshape(B * S, D)
    top_idx, top_w = route(h2, w_router, router_bias)
    routed = routed_experts(h2, top_idx, top_w.astype(h2.dtype), w_exp_gate, w_exp_up, w_exp_down)
    shared = (jax.nn.silu(h2 @ w_sh_gate) * (h2 @ w_sh_up)) @ w_sh_down
    return (routed + shared).reshape(B, S, D)


def setup_inputs(seed: int = 0) -> dict:
    key = jax.random.key(seed)
    ks = jax.random.split(key, 32)
    f32 = jnp.float32
    L = DEPTH

    def nrm(k, shape, sc):
        return jax.random.normal(k, shape, f32) * sc

    return {
        'x': nrm(ks[0], (BATCH, SEQ, D_MODEL), 1.0),
        'c': nrm(ks[1], (BATCH, D_MODEL), 1.0),
        'positions': jnp.tile(jnp.arange(SEQ, dtype=jnp.int32)[None, :], (BATCH, 1)),
        'w_ada': nrm(ks[2], (L, D_MODEL, 6 * D_MODEL), 0.5 * D_MODEL ** -0.5),
        'b_ada': nrm(ks[3], (L, 6 * D_MODEL), 0.01),
        'g_mix': 1.0 + nrm(ks[4], (L, D_MODEL), 0.02),
        'w_in': nrm(ks[5], (L, D_MODEL, IN_WIDTH), D_MODEL ** -0.5),
        'cmp_k_pe': nrm(ks[6], (L, CMP_BLOCK, HEAD_DIM), 0.1),
        'cmp_k_w1': nrm(ks[7], (L, CMP_BLOCK * HEAD_DIM, HEAD_DIM), (CMP_BLOCK * HEAD_DIM) ** -0.5),
        'cmp_k_w2': nrm(ks[8], (L, HEAD_DIM, HEAD_DIM), HEAD_DIM ** -0.5),
        'cmp_v_pe': nrm(ks[9], (L, CMP_BLOCK, HEAD_DIM), 0.1),
        'cmp_v_w1': nrm(ks[10], (L, CMP_BLOCK * HEAD_DIM, HEAD_DIM), (CMP_BLOCK * HEAD_DIM) ** -0.5),
        'cmp_v_w2': nrm(ks[11], (L, HEAD_DIM, HEAD_DIM), HEAD_DIM ** -0.5),
        'conv_w': nrm(ks[12], (L, CONV_KERNEL, 1, CONV_WIDTH), CONV_KERNEL ** -0.5),
        'conv_b': nrm(ks[13], (L, CONV_WIDTH), 0.01),
        'conv_ln_g': 1.0 + nrm(ks[14], (L, CONV_WIDTH), 0.02),
        'conv_ln_b': nrm(ks[15], (L, CONV_WIDTH), 0.01),
        'w_out': nrm(ks[16], (L, MIX_WIDTH, D_MODEL), MIX_WIDTH ** -0.5),
        'g_ffn': 1.0 + nrm(ks[17], (L, D_MODEL), 0.02),
        'w_router': nrm(ks[18], (L, D_MODEL, N_EXPERTS), D_MODEL ** -0.5),
        'router_bias': nrm(ks[19], (L, N_EXPERTS), 0.01),
        'w_exp_gate': nrm(ks[20], (L, N_EXPERTS, D_MODEL, EXPERT_HIDDEN), D_MODEL ** -0.5),
        'w_exp_up': nrm(ks[21], (L, N_EXPERTS, D_MODEL, EXPERT_HIDDEN), D_MODEL ** -0.5),
        'w_exp_down': nrm(ks[22], (L, N_EXPERTS, EXPERT_HIDDEN, D_MODEL), EXPERT_HIDDEN ** -0.5),
        'w_sh_gate': nrm(ks[23], (L, D_MODEL, SHARED_HIDDEN), D_MODEL ** -0.5),
        'w_sh_up': nrm(ks[24], (L, D_MODEL, SHARED_HIDDEN), D_MODEL ** -0.5),
        'w_sh_down': nrm(ks[25], (L, SHARED_HIDDEN, D_MODEL), SHARED_HIDDEN ** -0.5),
        'g_final': 1.0 + nrm(ks[26], (D_MODEL,), 0.02),
    }


def reference(x, c, positions, w_ada, b_ada, g_mix, w_in, cmp_k_pe, cmp_k_w1, cmp_k_w2,
              cmp_v_pe, cmp_v_w1, cmp_v_w2, conv_w, conv_b, conv_ln_g, conv_ln_b, w_out,
              g_ffn, w_router, router_bias, w_exp_gate, w_exp_up, w_exp_down,
              w_sh_gate, w_sh_up, w_sh_down, g_final):
    for l in range(DEPTH):
        mod = jax.nn.silu(c) @ w_ada[l] + b_ada[l]
        sh_m, sc_m, gt_m, sh_f, sc_f, gt_f = jnp.split(mod[:, None, :], 6, axis=-1)
        h = rms_norm(x, g_mix[l]) * (1.0 + sc_m) + sh_m
        x = x + gt_m * hybrid_mixer(h, positions, w_in[l], cmp_k_pe[l], cmp_k_w1[l],
                                    cmp_k_w2[l], cmp_v_pe[l], cmp_v_w1[l], cmp_v_w2[l],
                                    conv_w[l], conv_b[l], conv_ln_g[l], conv_ln_b[l], w_out[l])
        h = rms_norm(x, g_ffn[l]) * (1.0 + sc_f) + sh_f
        x = x + gt_f * moe_ffn(h, w_router[l], router_bias[l], w_exp_gate[l], w_exp_up[l],
                               w_exp_down[l], w_sh_gate[l], w_sh_up[l], w_sh_down[l])
    return rms_norm(x, g_final)
```

```python
import numpy as np
from contextlib import ExitStack
import concourse.bass as bass
import concourse.mybir as mybir
from concourse.bass_utils import run_bass_kernel_spmd

F32 = mybir.dt.float32
BF16 = mybir.dt.bfloat16
I32 = mybir.dt.int32
AF = mybir.ActivationFunctionType
ALU = mybir.AluOpType
AX = mybir.AxisListType

D = 2048
S = 2048
NB = 4
NCORES = 8
EPS = 1e-6
SCALE = 128 ** -0.5
PI = float(np.pi)
NEGBIG = -3.0e38


class Sched:
    NDMA = 40
    DMAPOOL = {'sp': (0, 16), 'pool': (16, 16), 'act': (32, 8)}

    def __init__(self, nc, es):
        self.nc = nc
        self.eng = {'pe': nc.tensor, 'act': nc.scalar, 'dve': nc.vector, 'pool': nc.gpsimd, 'sp': nc.sync}
        self.sem = {}
        self.cnt = {}
        for k in ['pe', 'act', 'dve', 'pool', 'cc']:
            self.sem[k] = es.enter_context(nc.semaphore('s_' + k))
            self.cnt[k] = 0
        for i in range(self.NDMA):
            k = 'd%d' % i
            self.sem[k] = es.enter_context(nc.semaphore('s_' + k))
            self.cnt[k] = 0
        self.dma_rr_q = {'sp': 0, 'pool': 0, 'act': 0}
        self.known = {k: {} for k in self.eng}
        self.last_w = {}
        self.readers = {}
        self.n_inst = 0

    def _need(self, reads, writes):
        need = {}

        def add(tok):
            if tok is None:
                return
            k, v = tok
            if need.get(k, 0) < v:
                need[k] = v
        for r in reads:
            add(self.last_w.get(r))
        for w in writes:
            add(self.last_w.get(w))
            for k, v in self.readers.get(w, {}).items():
                add((k, v))
        return need

    def _waits(self, e, need):
        kn = self.known[e]
        for k, v in need.items():
            if k == 'pe' and e == 'pe':
                continue
            if kn.get(k, 0) >= v:
                continue
            self.eng[e].wait_ge(self.sem[k], v)
            kn[k] = v

    def _record(self, tok, reads, writes):
        for r in reads:
            d = self.readers.setdefault(r, {})
            if d.get(tok[0], 0) < tok[1]:
                d[tok[0]] = tok[1]
        for w in writes:
            self.last_w[w] = tok
            self.readers[w] = {}

    def op(self, e, fn, reads=(), writes=()):
        pr = [k for k in reads if isinstance(k, str) and k.startswith('ps') and k not in writes]
        if pr:
            writes = list(writes) + pr
        self._waits(e, self._need(reads, writes))
        ins = fn(self.eng[e])
        ins.then_inc(self.sem[e], 1)
        self.cnt[e] += 1
        tok = (e, self.cnt[e])
        self._record(tok, reads, writes)
        self.n_inst += 1
        return tok

    def dma(self, q, out, in_, reads=(), writes=(), **kw):
        self._waits(q, self._need(reads, writes))
        lo, n = self.DMAPOOL[q]
        i = lo + self.dma_rr_q[q]
        self.dma_rr_q[q] = (self.dma_rr_q[q] + 1) % n
        k = 'd%d' % i
        if self.cnt[k] > 0 and self.known[q].get(k, 0) < self.cnt[k]:
            self.eng[q].wait_ge(self.sem[k], self.cnt[k])
            self.known[q][k] = self.cnt[k]
        ins = self.eng[q].dma_start(out=out, in_=in_, **kw)
        ins.then_inc(self.sem[k], 16)
        self.cnt[k] += 16
        tok = (k, self.cnt[k])
        self._record(tok, reads, writes)
        self.n_inst += 1
        return tok

    def collective(self, kind, alu, ins, outs, groups, reads=(), writes=()):
        self._waits('pool', self._need(reads, writes))
        ins_ = self.nc.gpsimd.collective_compute(kind, alu, replica_groups=groups, ins=ins, outs=outs)
        ins_.then_inc(self.sem['cc'])
        self.cnt['cc'] += 1
        tok = ('cc', self.cnt['cc'])
        self._record(tok, reads, writes)
        return tok

    def barrier(self):
        need = {k: v for k, v in self.cnt.items() if v > 0}
        for e in self.eng:
            kn = self.known[e]
            for k, v in need.items():
                if kn.get(k, 0) >= v:
                    continue
                self.eng[e].wait_ge(self.sem[k], v)
                kn[k] = v


def _pk(w, n=None):
    K, N = w.shape
    return np.ascontiguousarray(w.reshape(K // 128, 128, N).transpose(1, 0, 2))


def _consts(g):
    c = {}
    c['ident'] = np.eye(128, dtype=np.float32)
    r = np.arange(128)
    c['tri'] = (r[:, None] <= r[None, :]).astype(np.float32)
    c['triu'] = (r[:, None] > r[None, :]).astype(np.float32)
    pm = np.zeros((128, 128), np.float32)
    for dd in range(64):
        pm[dd + 64, dd] = -1.0
        pm[dd, dd + 64] = 1.0
    c['pm'] = pm
    off = 1024 * (1 - g)
    j = np.arange(1024)
    tctx = 1024 + j
    cc = np.arange(127)
    mk = ((cc[:, None] * 16 + 31) <= tctx[None, :]) & ((cc[:, None] * 16) >= off)
    c['maskc'] = np.zeros((128, 1024), np.float32)
    c['maskc'][:127] = mk.astype(np.float32)
    cs = cc * 16
    js = np.arange(32) * 64
    ovl = ((cs[:, None] < js[None, :] + 64) & (cs[:, None] + 32 > js[None, :])).astype(np.float32)
    o33 = np.zeros((128, 33), np.float32)
    o33[:127, :32] = ovl
    o33[:127, 32] = 1.0
    c['ovl'] = o33
    jbc = np.arange(32)
    jbt = jbc - off // 64
    ttrue = tctx - off
    cur = (ttrue // 64)[:, None]
    exist = (jbt >= 0)[None, :]
    forced = exist & ((jbt[None, :] == 0) | (jbt[None, :] == cur) | (jbt[None, :] == cur - 1))
    causal = exist & (jbt[None, :] * 64 <= ttrue[:, None])
    cm = (causal & ~forced).astype(np.float32)
    fb = np.where(forced, 1e30, np.where(causal, 0.0, -1e30)).astype(np.float32)
    c['cm'] = np.ascontiguousarray(cm.reshape(8, 128, 32).transpose(1, 0, 2))
    c['fb'] = np.ascontiguousarray(fb.reshape(8, 128, 32).transpose(1, 0, 2))
    ex = np.zeros((128, S), np.float32)
    ex[:32] = (np.arange(S)[None, :] // 64 == jbc[:, None]).astype(np.float32)
    c['ex'] = ex
    half = 64
    inv = (10000.0 ** (-np.arange(half, dtype=np.float32) / half)).astype(np.float32)
    c['invf'] = np.concatenate([inv, inv]).reshape(128, 1).astype(np.float32)
    c['halo'] = np.full((128, 1), float(g), np.float32)
    return c


def prep_stage1(inp):
    f = lambda k: np.asarray(inp[k])
    x = f('x'); c = f('c'); pos = f('positions')
    w_in = f('w_in')[0]
    vec16 = lambda v: np.ascontiguousarray(v.reshape(16, 128).T).astype(np.float32)
    vec8 = lambda v: np.ascontiguousarray(v.reshape(8, 128).T).astype(np.float32)
    sh = {}
    sh['wada'] = _pk(f('w_ada')[0])
    sh['bada'] = np.ascontiguousarray(f('b_ada')[0].reshape(96, 128).T)
    sh['gmix'] = vec16(f('g_mix')[0]); sh['gffn'] = vec16(f('g_ffn')[0])
    for nm, key in (('k', 'cmp_k'), ('v', 'cmp_v')):
        sh['c%sw1' % nm] = np.ascontiguousarray(f(key + '_w1')[0].reshape(32, 128, 128).transpose(1, 0, 2))
        sh['c%sw2' % nm] = np.ascontiguousarray(f(key + '_w2')[0])
        sh['c%spe' % nm] = np.ascontiguousarray(f(key + '_pe')[0].T)
    sh['convw'] = np.ascontiguousarray(f('conv_w')[0][:, 0, :].reshape(31, 8, 128).transpose(2, 1, 0))
    sh['convb'] = vec8(f('conv_b')[0]); sh['lng'] = vec8(f('conv_ln_g')[0]); sh['lnb'] = vec8(f('conv_ln_b')[0])
    sh['wq'] = _pk(w_in[:, 0:1024])
    kcols = []
    for gi in range(2):
        for base in (1024, 1536, 2048, 1280):
            kcols.append(np.arange(base + 128 * gi, base + 128 * gi + 128))
    sh['wkt'] = _pk(w_in[:, np.concatenate(kcols)])
    vcols = [np.arange(1792, 1792 + 256), np.arange(2304, 2304 + 256), np.arange(2560, 2584)]
    sh['wvn'] = _pk(w_in[:, np.concatenate(vcols)])
    sh['wglu'] = _pk(w_in[:, 2584:2584 + 2048])
    sh['wo'] = _pk(f('w_out')[0])
    sh['wr'] = _pk(f('w_router')[0])
    sh['rb'] = np.ascontiguousarray(np.broadcast_to(f('router_bias')[0][None, :], (128, 256))).astype(np.float32)
    cons = [_consts(0), _consts(1)]
    maps = []
    for r in range(NCORES):
        b, g = r // 2, r % 2
        m = dict(sh)
        m.update(cons[g])
        m['cTb'] = np.ascontiguousarray(c[b].reshape(16, 128).T).astype(np.float32)
        xc = np.zeros((S, D), np.float32)
        pc = np.zeros((S,), np.int32)
        if g == 1:
            xc[:] = x[b]; pc[:] = pos[b]
        else:
            xc[1024:] = x[b, :1024]; pc[1024:] = pos[b, :1024]
        m['xctx'] = xc
        m['posr'] = np.ascontiguousarray(np.broadcast_to(pc[None, :], (128, S))).astype(np.int32)
        maps.append(m)
    return maps


def _ak(x):
    if isinstance(x, tuple):
        ap, k = x
        return ap, (list(k) if isinstance(k, list) else [k])
    return x, [x.name]


class Ops:
    def __init__(self, S_):
        self.S = S_

    def act(self, out, in_, func, bias=None, scale=None, accum=None, eng='act'):
        o, ok = _ak(out); i, ik = _ak(in_)
        rk = list(ik); wk = list(ok); kw = {}
        if bias is not None:
            if isinstance(bias, (float, int)):
                kw['bias'] = float(bias)
            else:
                b, bk = _ak(bias); kw['bias'] = b; rk += bk
        if scale is not None:
            if isinstance(scale, (float, int)):
                kw['scale'] = float(scale)
            else:
                s, sk = _ak(scale); kw['scale'] = s; rk += sk
        if accum is not None:
            a, akk = _ak(accum); kw['accum_out'] = a; wk += akk
        return self.S.op('act', lambda e: e.activation(out=o, in_=i, func=func, **kw), rk, wk)

    def tt(self, eng, out, in0, in1, op):
        o, ok = _ak(out); a, ak = _ak(in0); b, bk = _ak(in1)
        return self.S.op(eng, lambda e: e.tensor_tensor(out=o, in0=a, in1=b, op=op), ak + bk, ok)

    def ts(self, eng, out, in0, s1, s2, op0, op1=None, accum=None):
        o, ok = _ak(out); a, ak = _ak(in0)
        rk = list(ak); wk = list(ok)
        if not isinstance(s1, (float, int)):
            s1, k1 = _ak(s1); rk += k1
        if s2 is not None and not isinstance(s2, (float, int)):
            s2, k2 = _ak(s2); rk += k2
        kw = {}
        if op1 is not None:
            kw['op1'] = op1
        if accum is not None:
            ac, ack = _ak(accum); kw['accum_out'] = ac; wk += ack
        return self.S.op(eng, lambda e: e.tensor_scalar(out=o, in0=a, scalar1=s1, scalar2=s2, op0=op0, **kw), rk, wk)

    def stt(self, eng, out, in0, scalar, in1, op0, op1):
        o, ok = _ak(out); a, ak = _ak(in0); b, bk = _ak(in1)
        rk = ak + bk
        if not isinstance(scalar, (float, int)):
            scalar, sk = _ak(scalar); rk = rk + sk
        return self.S.op(eng, lambda e: e.scalar_tensor_tensor(out=o, in0=a, scalar=scalar, in1=b, op0=op0, op1=op1), rk, ok)

    def copy(self, eng, out, in_):
        o, ok = _ak(out); i, ik = _ak(in_)
        if eng == 'act':
            return self.S.op('act', lambda e: e.activation(out=o, in_=i, func=AF.Copy), ik, ok)
        return self.S.op(eng, lambda e: e.tensor_copy(out=o, in_=i), ik, ok)

    def memset(self, eng, out, val):
        o, ok = _ak(out)
        return self.S.op(eng, lambda e: e.memset(o, val), [], ok)

    def recip(self, out, in_):
        o, ok = _ak(out); i, ik = _ak(in_)
        return self.S.op('dve', lambda e: e.reciprocal(out=o, in_=i), ik, ok)

    def mm(self, out, lhsT, rhs, start=True, stop=True):
        o, ok = _ak(out); l, lk = _ak(lhsT); r, rk = _ak(rhs)
        return self.S.op('pe', lambda e: e.matmul(o, lhsT=l, rhs=r, start=start, stop=stop), lk + rk, ok)

    def tr(self, out, in_, ident):
        o, ok = _ak(out); i, ik = _ak(in_); d, dk = _ak(ident)
        return self.S.op('pe', lambda e: e.transpose(out=o, in_=i, identity=d), ik + dk, ok)

    def rsum(self, out, in_):
        o, ok = _ak(out); i, ik = _ak(in_)
        return self.S.op('dve', lambda e: e.reduce_sum(out=o, in_=i, axis=AX.X), ik, ok)

    def max8(self, out, in_):
        o, ok = _ak(out); i, ik = _ak(in_)
        return self.S.op('dve', lambda e: e.max(out=o, in_=i), ik, ok)

    def mrep(self, out, rep, vals, imm):
        o, ok = _ak(out); r, rk = _ak(rep); v, vk = _ak(vals)
        return self.S.op('dve', lambda e: e.match_replace(out=o, in_to_replace=r, in_values=v, imm_value=imm), rk + vk, ok)

    def dma(self, q, out, in_, **kw):
        o, ok = _ak(out); i, ik = _ak(in_)
        return self.S.dma(q, o, i, reads=ik, writes=ok, **kw)


class Ctx:
    def __init__(self, specs):
        self.nc = bass.Bass("TRN2", target_bir_lowering=False)
        self.specs = specs
        self.in_names = []
        self._decl = {}
        self.outs = {}

    def IN(self, name):
        if name not in self._decl:
            shp, dt = self.specs[name]
            self.in_names.append(name)
            self._decl[name] = self.nc.dram_tensor(name, list(shp), dt, kind="ExternalInput").ap()
        return self._decl[name]

    def OUT(self, name, shape, dt=F32):
        self.outs[name] = self.nc.dram_tensor(name, list(shape), dt, kind="ExternalOutput").ap()
        return self.outs[name]


S1_SPECS = {
    'ident': ([128, 128], F32), 'tri': ([128, 128], F32), 'triu': ([128, 128], F32), 'pm': ([128, 128], F32),
    'maskc': ([128, 1024], F32), 'ovl': ([128, 33], F32), 'cm': ([128, 8, 32], F32), 'fb': ([128, 8, 32], F32),
    'ex': ([128, S], F32), 'invf': ([128, 1], F32), 'halo': ([128, 1], F32),
    'cTb': ([128, 16], F32), 'wada': ([128, 16, 12288], F32), 'bada': ([128, 96], F32),
    'gmix': ([128, 16], F32), 'gffn': ([128, 16], F32),
    'ckw1': ([128, 32, 128], F32), 'ckw2': ([128, 128], F32), 'ckpe': ([128, 32], F32),
    'cvw1': ([128, 32, 128], F32), 'cvw2': ([128, 128], F32), 'cvpe': ([128, 32], F32),
    'convw': ([128, 8, 31], F32), 'convb': ([128, 8], F32), 'lng': ([128, 8], F32), 'lnb': ([128, 8], F32),
    'wq': ([128, 16, 1024], F32), 'wkt': ([128, 16, 1024], F32), 'wvn': ([128, 16, 536], F32),
    'wglu': ([128, 16, 2048], F32), 'wo': ([128, 16, D], F32), 'wr': ([128, 16, 256], F32), 'rb': ([128, 256], F32),
    'xctx': ([S, D], F32), 'posr': ([128, S], I32),
}


def emit_stage1(C, Sd, O, es, ps, cfg, ex_out):
    nc = C.nc
    IN = C.IN
    dbg = set(cfg.get('dbg', ()))
    stop = cfg.get('stop', 'end')
    fin = []
    sb = lambda st, name, shape, dt=F32: st.enter_context(nc.sbuf_tensor('t_' + name, list(shape), dt))

    def dbg_dump(name, shape, src_ap, dt=F32):
        Sd.barrier()
        t_ = C.OUT('dbg_' + name, shape, dt)
        fin.append(O.dma('sp', t_, src_ap))

    ident_f = sb(es, 'ident_f', [128, 128]); ident_b = sb(es, 'ident_b', [128, 128], BF16)
    modown = sb(es, 'modown', [128, 96]); gm_m = sb(es, 'gm_m', [128, 16]); gm_f = sb(es, 'gm_f', [128, 16])
    halo = sb(es, 'halo', [128, 1])
    O.dma('sp', ident_f[:], IN('ident')[:, :])
    O.copy('dve', ident_b[:], ident_f[:])
    O.dma('sp', halo[:], IN('halo')[:, :])
    modT_d = ex_out['modT']

    with ExitStack() as ph:
        wsl = [sb(ph, 'wsl%d' % i, [128, 16, 768]) for i in range(2)]
        cTb = sb(ph, 'cTb', [128, 16]); scT = sb(ph, 'scT', [128, 16])
        bada = sb(ph, 'bada', [128, 96]); gmix = sb(ph, 'gmix', [128, 16]); gffn = sb(ph, 'gffn', [128, 16])
        modT = sb(ph, 'modT', [96, 128])
        O.dma('sp', cTb[:], IN('cTb')[:, :]); O.dma('sp', bada[:], IN('bada')[:, :])
        O.dma('sp', gmix[:], IN('gmix')[:, :]); O.dma('sp', gffn[:], IN('gffn')[:, :])
        O.act(scT[:], cTb[:], AF.Silu)
        for sl in range(16):
            w_ = wsl[sl % 2]
            O.dma('sp' if sl % 2 == 0 else 'act', w_[:], IN('wada')[:, :, sl * 768:(sl + 1) * 768])
            for jj in range(6):
                q = sl * 6 + jj
                for kc in range(16):
                    O.mm(ps[0][:, q:q + 1], w_[:, kc, jj * 128:(jj + 1) * 128], scT[:, kc:kc + 1],
                         start=(kc == 0), stop=(kc == 15))
        O.tt('dve', modown[:], ps[0][:, 0:96], bada[:], ALU.add)
        O.stt('dve', gm_m[:], modown[:, 16:32], 1.0, gmix[:], ALU.add, ALU.mult)
        O.stt('dve', gm_f[:], modown[:, 64:80], 1.0, gffn[:], ALU.add, ALU.mult)
        O.tr(ps[1][0:96, 0:128], modown[:, 0:96], ident_f[:])
        O.copy('dve', modT[:], ps[1][0:96, 0:128])
        O.dma('sp', modT_d, modT[:])
        if 'mod' in dbg:
            dbg_dump('mod', [128, 96], modown[:])
        Sd.barrier()
    sh_m = modown[:, 0:16]; sh_f = modown[:, 48:64]

    def finish():
        need = {}
        for k, v in fin:
            if need.get(k, 0) < v:
                need[k] = v
        Sd._waits('sp', need)
        Sd.barrier()

    if stop == 'A':
        finish()
        return True

    def gt_row(dst, q0):
        O.dma('sp', dst[:], (modT_d[q0:q0 + 16, :].rearrange("(o q) p -> o (q p)", o=1).partition_broadcast(128), modT_d.name))

    def norm_T(ph, load_fn, ntiles, gm, shv, dstT, dkey, extra=None, pfx='', post=None):
        xn = [sb(ph, pfx + 'xn%d' % i, [128, D]) for i in range(2)]
        junk = sb(ph, pfx + 'junk', [128, D], BF16)
        ssq = sb(ph, pfx + 'ssq', [128, ntiles]); rs = sb(ph, pfx + 'rs', [128, ntiles])
        for t in range(ntiles):
            xt = load_fn(t)
            xnt = xn[t % 2]
            kq = (pfx + 'ssq', t); kr = (pfx + 'rs', t)
            O.act(junk[:], xt, AF.Square, accum=(ssq[:, t:t + 1], kq))
            O.ts('dve', (rs[:, t:t + 1], kr), (ssq[:, t:t + 1], kq), 1.0 / D, EPS, ALU.mult, ALU.add)
            O.act((rs[:, t:t + 1], kr), (rs[:, t:t + 1], kr), AF.Sqrt)
            O.recip((rs[:, t:t + 1], kr), (rs[:, t:t + 1], kr))
            O.act(xnt[:], xt, AF.Copy, scale=(rs[:, t:t + 1], kr))
            for g4 in range(4):
                bank = ps[(t * 4 + g4) % 4]
                for j in range(4):
                    dc = g4 * 4 + j
                    O.tr(bank[:, j * 128:(j + 1) * 128], xnt[:, dc * 128:(dc + 1) * 128], ident_f[:])
                for j in range(4):
                    dc = g4 * 4 + j
                    dst = (dstT(t, dc), (dkey, t, dc))
                    if g4 % 2 == 0:
                        O.act(dst, bank[:, j * 128:(j + 1) * 128], AF.Identity, scale=gm[:, dc:dc + 1], bias=shv[:, dc:dc + 1])
                    else:
                        O.ts('dve', dst, bank[:, j * 128:(j + 1) * 128], gm[:, dc:dc + 1], shv[:, dc:dc + 1], ALU.mult, ALU.add)
                    if extra is not None:
                        extra(t, dc, bank[:, j * 128:(j + 1) * 128], g4 % 2 == 0)
            if post is not None:
                post(t)

    oT_d = nc.dram_tensor('oT_d', [16, 128, 1024], BF16)
    ex_out['_oTd'] = oT_d
    stopped = [False]
    with ExitStack() as pm_:
        qT = sb(pm_, 'qT', [128, 8, 1024], BF16)
        kT = sb(pm_, 'kT', [128, 8, S], BF16)
        Vs = sb(pm_, 'Vs', [128, 16, 2, 132], BF16); Vw = sb(pm_, 'Vw', [128, 16, 2, 132], BF16)
        G = sb(pm_, 'G', [128, 8, 24])
        pm_b = sb(pm_, 'pm_b', [128, 128], BF16)
        y = sb(pm_, 'y', [128, 8, 1152], BF16)
        cosC = sb(pm_, 'cosC', [128, 128], BF16); sinC = sb(pm_, 'sinC', [128, 128], BF16)
        pt_ = pm_.enter_context(ExitStack())
        cosT = sb(pt_, 'cosT', [128, S], BF16); sinT = sb(pt_, 'sinT', [128, S], BF16)
        O.memset('pool', Vs[:, :, :, 128:132], 1.0); O.memset('pool', Vw[:, :, :, 128:132], 1.0)
        with ExitStack() as ph:
            tmp = sb(ph, 'pmtmp', [128, 128])
            O.dma('sp', tmp[:], IN('pm')[:, :]); O.copy('dve', pm_b[:], tmp[:])
            posi = sb(ph, 'posi', [128, S], I32); ang = sb(ph, 'ang', [128, S]); invf = sb(ph, 'invf', [128, 1])
            ta = sb(ph, 'ta', [128, S]); tb = sb(ph, 'tb', [128, S]); ki = sb(ph, 'ki', [128, S], I32)
            O.dma('sp', posi[:], IN('posr')[:, :]); O.dma('sp', invf[:], IN('invf')[:, :])
            O.copy('dve', ang[:], posi[:])
            O.ts('dve', ang[:], ang[:], invf[:, 0:1], None, ALU.mult)
            for (shift, table) in ((0.0, sinT), (PI / 2, cosT)):
                O.ts('dve', ta[:], ang[:], shift, 1.0 / (2 * PI), ALU.add, ALU.mult)
                O.copy('dve', ki[:], ta[:]); O.copy('dve', tb[:], ki[:])
                O.ts('dve', ta[:], ta[:], 2 * PI, None, ALU.mult)
                O.stt('dve', ta[:], tb[:], -2 * PI, ta[:], ALU.mult, ALU.add)
                O.ts('dve', tb[:], ta[:], PI, None, ALU.is_gt)
                O.stt('dve', ta[:], tb[:], -2 * PI, ta[:], ALU.mult, ALU.add)
                O.ts('dve', tb[:], ta[:], -PI, None, ALU.is_lt)
                O.stt('dve', ta[:], tb[:], 2 * PI, ta[:], ALU.mult, ALU.add)
                O.ts('dve', ta[:], ta[:], PI, -PI, ALU.min, ALU.max)
                O.act(table[:], ta[:], AF.Sin)
            csl_ = slice(31, 31 + 16 * 126 + 1, 16)
            O.copy('dve', cosC[:, 0:127], cosT[:, csl_]); O.copy('dve', sinC[:, 0:127], sinT[:, csl_])
            if 'tab' in dbg:
                dbg_dump('cosT', [128, S], cosT[:], BF16); dbg_dump('sinT', [128, S], sinT[:], BF16)
            Sd.barrier()
        if stop == 'tab':
            finish()
            return True

        with ExitStack() as ph:
            hT = sb(ph, 'hT', [128, 16, S], BF16)
            with ExitStack() as ph2:
                xbuf = [sb(ph2, 'xbuf%d' % i, [128, D]) for i in range(2)]

                def load_ctx(t):
                    O.dma('sp', xbuf[t % 2][:], IN('xctx')[t * 128:(t + 1) * 128, :])
                    return xbuf[t % 2][:]
                norm_T(ph2, load_ctx, 16, gm_m, sh_m, lambda t, dc: hT[:, dc, t * 128:(t + 1) * 128], 'hT')
                Sd.barrier()
            if 'hT' in dbg:
                t_ = C.OUT('dbg_hT', [128, 16, S], BF16)
                for dc in range(16):
                    fin.append(O.dma('sp', t_[:, dc, :], (hT[:, dc, :], [('hT', t, dc) for t in range(16)])))
            if stop == 'norm':
                finish()
                return True
            hk = lambda t0, t1: [('hT', t, dc) for t in range(t0, t1) for dc in range(16)]
            rawb0 = sb(ph, 'rawb0', [128, 512], BF16); r10 = sb(ph, 'r1_0', [128, 512]); r20 = sb(ph, 'r2_0', [128, 512])
            rawb = [rawb0, rawb0]; r1 = [r10, r10]; r2 = [r20, r20]
            wb1 = sb(ph, 'wb0', [128, 16, 512], BF16)
            wb = [wb1, wb1]
            cnt = [0]

            def rope_evac(raw_ps, sw_ps, dst, c0, n, tcos, tsin):
                i = cnt[0] % 2; cnt[0] += 1
                O.copy('act', rawb[i][:, 0:n], raw_ps)
                O.mm(sw_ps, pm_b[:], rawb[i][:, 0:n])
                O.tt('dve', r1[i][:, 0:n], raw_ps, tcos, ALU.mult)
                O.tt('dve', r2[i][:, 0:n], sw_ps, tsin, ALU.mult)
                O.tt('pool', dst, r1[i][:, 0:n], r2[i][:, 0:n], ALU.add)

            def load_w(i, name, c0, n):
                O.dma('pool', wb[i][:, :, 0:n], IN(name)[:, :, c0:c0 + n])
            wi = 0
            for half in range(2):
                load_w(wi % 2, 'wq', half * 512, 512)
                for hh in range(4):
                    h8 = half * 4 + hh
                    for tc in range(2):
                        bank = ps[4 + (h8 * 2 + tc) % 2]; bank2 = ps[6 + (h8 * 2 + tc) % 2]
                        t0 = 8 + tc * 4
                        for kc in range(16):
                            O.mm(bank[:, :], wb[wi % 2][:, kc, hh * 128:(hh + 1) * 128],
                                 (hT[:, kc, t0 * 128:(t0 + 4) * 128], [('hT', t, kc) for t in range(t0, t0 + 4)]),
                                 start=(kc == 0), stop=(kc == 15))
                        if 'q0' in dbg and h8 == 0 and tc == 0:
                            dq = sb(ph, 'dq', [128, 512])
                            O.copy('dve', dq[:], bank[:, :])
                            dbg_dump('q0raw', [128, 512], dq[:])
                            t_ = C.OUT('dbg_wb', [128, 16, 512], BF16)
                            for kc_ in range(16):
                                fin.append(O.dma('sp', t_[:, kc_, :], wb[0][:, kc_, :]))
                        rope_evac(bank[:, :], bank2[:, :], (qT[:, h8, tc * 512:(tc + 1) * 512], ('qT', h8, tc)), 0, 512,
                                  cosT[:, t0 * 128:(t0 + 4) * 128], sinT[:, t0 * 128:(t0 + 4) * 128])
                wi += 1
            if stop == 'q':
                dbg_dump('qT', [128, 8, 1024], qT[:], BF16)
                finish()
                return True
            for half in range(2):
                load_w(wi % 2, 'wkt', half * 512, 512)
                for kk in range(4):
                    k8 = half * 4 + kk
                    kind = k8 % 4
                    for tc in range(4):
                        bank = ps[4 + (k8 * 4 + tc) % 2]; bank2 = ps[6 + (k8 * 4 + tc) % 2]
                        t0 = tc * 4
                        for kc in range(16):
                            O.mm(bank[:, :], wb[wi % 2][:, kc, kk * 128:(kk + 1) * 128],
                                 (hT[:, kc, t0 * 128:(t0 + 4) * 128], [('hT', t, kc) for t in range(t0, t0 + 4)]),
                                 start=(kc == 0), stop=(kc == 15))
                        dst = (kT[:, k8, tc * 512:(tc + 1) * 512], ('kT', k8, tc))
                        if kind in (1, 2):
                            rope_evac(bank[:, :], bank2[:, :], dst, 0, 512,
                                      cosT[:, t0 * 128:(t0 + 4) * 128], sinT[:, t0 * 128:(t0 + 4) * 128])
                        else:
                            O.copy('act', dst, bank[:, :])
                wi += 1
            for (c0, n) in ((0, 512), (512, 24)):
                load_w(wi % 2, 'wvn', c0, n)
                for t in range(16):
                    if n == 24 and t < 8:
                        continue
                    bank = ps[4 + t % 2]
                    for kc in range(16):
                        O.mm(bank[:, 0:n], (hT[:, kc, t * 128:(t + 1) * 128], ('hT', t, kc)), wb[wi % 2][:, kc, 0:n],
                             start=(kc == 0), stop=(kc == 15))
                    if n == 512:
                        O.copy('act', (Vs[:, t, :, 0:128], ('Vs', t)), bank[:, 0:256].rearrange("p (g d) -> p g d", g=2))
                        O.copy('dve', (Vw[:, t, :, 0:128], ('Vw', t)), bank[:, 256:512].rearrange("p (g d) -> p g d", g=2))
                    else:
                        O.act((G[:, t - 8, :], ('G', t - 8)), bank[:, 0:24], AF.Sigmoid)
                wi += 1
            if 'proj' in dbg:
                dbg_dump('qT', [128, 8, 1024], qT[:], BF16)
                dbg_dump('kT', [128, 8, S], kT[:], BF16)
                dbg_dump('Vs', [128, 16, 2, 132], Vs[:], BF16)
                dbg_dump('G', [128, 8, 24], G[:])
            if stop == 'proj':
                finish()
                return True

            sg0 = sb(ph, 'sg0', [128, 512], BF16)
            sg = [sg0, sg0]
            TCH = ((896, 512), (1408, 512), (1920, 128))
            for cch in range(8):
                O.dma('pool', wb[wi % 2][:, :, 0:128], IN('wglu')[:, :, cch * 128:(cch + 1) * 128])
                O.dma('pool', wb[wi % 2][:, :, 128:256], IN('wglu')[:, :, 1024 + cch * 128:1024 + (cch + 1) * 128])
                for ti, (c0, n) in enumerate(TCH):
                    ba = ps[4 + (cch * 3 + ti) % 2]; bb = ps[6 + (cch * 3 + ti) % 2]
                    keys = lambda kc: [('hT', t, kc) for t in range(c0 // 128, (c0 + n) // 128)]
                    for kc in range(16):
                        O.mm(ba[:, 0:n], wb[wi % 2][:, kc, 0:128], (hT[:, kc, c0:c0 + n], keys(kc)), start=(kc == 0), stop=(kc == 15))
                    for kc in range(16):
                        O.mm(bb[:, 0:n], wb[wi % 2][:, kc, 128:256], (hT[:, kc, c0:c0 + n], keys(kc)), start=(kc == 0), stop=(kc == 15))
                    s_ = sg[(cch * 3 + ti) % 2]
                    O.act(s_[:, 0:n], bb[:, 0:n], AF.Sigmoid)
                    O.tt('dve', (y[:, cch, c0 - 896:c0 - 896 + n], ('y', cch, ti)), ba[:, 0:n], s_[:, 0:n], ALU.mult)
                O.ts('pool', (y[:, cch, 0:128], ('y', cch, 0)), (y[:, cch, 0:128], ('y', cch, 0)), halo[:, 0:1], None, ALU.mult)
                wi += 1
            Sd.barrier()
        pt_.close()
        ex_out['_state'] = dict(qT=qT, kT=kT, Vs=Vs, Vw=Vw, G=G, cosT=cosC, sinT=sinC, y=y, pm_b=pm_b,
                                ident_f=ident_f, ident_b=ident_b, halo=halo, modown=modown, gm_f=gm_f, sh_f=sh_f,
                                fin=fin, finish=finish, gt_row=gt_row, norm_T=norm_T, sb=sb, dbg_dump=dbg_dump, pm_=pm_)
        stopped[0] = emit_stage1b(C, Sd, O, es, ps, cfg, ex_out)
    if stopped[0]:
        return True
    return emit_stage1c(C, Sd, O, es, ps, cfg, ex_out)


def emit_stage1b(C, Sd, O, es, ps, cfg, ex_out):
    nc = C.nc
    IN = C.IN
    st = ex_out['_state']
    qT, kT, Vs, Vw, G, cosT, sinT, y, pm_b = (st[k] for k in ('qT', 'kT', 'Vs', 'Vw', 'G', 'cosT', 'sinT', 'y', 'pm_b'))
    ident_f, ident_b, halo, modown, gm_f, sh_f = (st[k] for k in ('ident_f', 'ident_b', 'halo', 'modown', 'gm_f', 'sh_f'))
    fin, finish, gt_row, norm_T, sb, dbg_dump, pm_ = (st[k] for k in ('fin', 'finish', 'gt_row', 'norm_T', 'sb', 'dbg_dump', 'pm_'))
    dbg = set(cfg.get('dbg', ()))
    stop = cfg.get('stop', 'end')
    oT_d = ex_out['_oTd']

    with ExitStack() as ph:
        convw = sb(ph, 'convw', [128, 8, 31]); convb = sb(ph, 'convb', [128, 8])
        lng = sb(ph, 'lng', [128, 8]); lnb = sb(ph, 'lnb', [128, 8])
        acc = sb(ph, 'cacc', [128, 8, 1024]); ones_f = sb(ph, 'ones_f', [128, 128])
        sq = [sb(ph, 'sq%d' % i, [128, 1024]) for i in range(2)]
        mean = sb(ph, 'mean', [128, 1024]); rstd = sb(ph, 'rstd', [128, 1024])
        oct_ = [sb(ph, 'oct%d' % i, [128, 1024], BF16) for i in range(2)]
        O.dma('sp', convw[:], IN('convw')[:, :, :]); O.dma('sp', convb[:], IN('convb')[:, :])
        O.dma('sp', lng[:], IN('lng')[:, :]); O.dma('sp', lnb[:], IN('lnb')[:, :])
        O.memset('dve', ones_f[:], 1.0)
        for cch in range(8):
            e = 'dve'
            ykeys = [('y', cch, ti) for ti in range(3)]
            a_c = (acc[:, cch, :], ('cacc', cch))
            O.ts(e, a_c, (y[:, cch, 98:98 + 1024], ykeys), convw[:, cch, 0:1], convb[:, cch:cch + 1], ALU.mult, ALU.add)
            for k in range(1, 31):
                O.stt(e, a_c, (y[:, cch, 98 + k:98 + k + 1024], ykeys), convw[:, cch, k:k + 1], a_c, ALU.mult, ALU.add)
        for cch in range(8):
            a_c = (acc[:, cch, :], ('cacc', cch))
            s_ = sq[cch % 2]
            O.act(s_[:], a_c, AF.Square)
            for tc in range(2):
                O.mm(ps[tc][:, :], ones_f[:], (acc[:, cch, tc * 512:(tc + 1) * 512], ('cacc', cch)), start=(cch == 0), stop=(cch == 7))
                O.mm(ps[2 + tc][:, :], ones_f[:], s_[:, tc * 512:(tc + 1) * 512], start=(cch == 0), stop=(cch == 7))
        for tc in range(2):
            sl = slice(tc * 512, (tc + 1) * 512)
            O.act(mean[:, sl], ps[tc][:, :], AF.Copy, scale=1.0 / 1024)
            O.tt('dve', rstd[:, sl], mean[:, sl], mean[:, sl], ALU.mult)
            O.stt('dve', rstd[:, sl], ps[2 + tc][:, :], 1.0 / 1024, rstd[:, sl], ALU.mult, ALU.subtract)
            O.ts('dve', rstd[:, sl], rstd[:, sl], EPS, None, ALU.add)
            O.act(rstd[:, sl], rstd[:, sl], AF.Sqrt)
            O.recip(rstd[:, sl], rstd[:, sl])
        for cch in range(8):
            e = 'dve' if cch % 2 == 0 else 'pool'
            a_c = (acc[:, cch, :], ('cacc', cch))
            O.tt(e, a_c, a_c, mean[:], ALU.subtract)
            O.tt(e, a_c, a_c, rstd[:], ALU.mult)
            oc_t = oct_[cch % 2]
            O.act(oc_t[:], a_c, AF.Silu, scale=lng[:, cch:cch + 1], bias=lnb[:, cch:cch + 1])
            O.dma('sp', (oT_d.ap()[8 + cch], ('oTd', 8 + cch)), oc_t[:])
        if 'conv' in dbg:
            Sd.barrier()
            t_ = C.OUT('dbg_ocT', [8, 128, 1024], BF16)
            fin.append(O.dma('sp', t_, oT_d.ap()[8:16]))
        Sd.barrier()
    if stop == 'conv':
        finish()
        return True

    kcc = [sb(pm_, 'kcc%d' % gi, [128, 128], BF16) for gi in range(2)]
    Vc = [sb(pm_, 'Vc%d' % gi, [128, 168], BF16) for gi in range(2)]
    with ExitStack() as ph:
        w1 = sb(ph, 'cw1', [128, 32, 128], BF16); w2 = sb(ph, 'cw2', [128, 128], BF16); pe = sb(ph, 'cpe', [128, 32], BF16)
        bias = sb(ph, 'cbias', [128, 1]); a1 = sb(ph, 'ca1', [128, 128], BF16)
        ovl_f = sb(ph, 'ovl_f', [128, 33])
        rawb = sb(ph, 'crawb', [128, 128], BF16); r1 = sb(ph, 'cr1', [128, 128]); r2 = sb(ph, 'cr2', [128, 128])
        O.dma('sp', ovl_f[:], IN('ovl')[:, :])
        for gi in range(2):
            O.memset('dve', Vc[gi][:], 0.0); O.memset('dve', kcc[gi][:], 0.0)
        csl = slice(31, 31 + 16 * 126 + 1, 16)
        for kind, nm in ((0, 'k'), (3, 'v')):
            O.dma('pool', w1[:], IN('c%sw1' % nm)[:, :, :]); O.dma('pool', w2[:], IN('c%sw2' % nm)[:, :])
            O.dma('pool', pe[:], IN('c%spe' % nm)[:, :])
            for l in range(32):
                O.mm(ps[1][:, 0:1], w1[:, l, :], pe[:, l:l + 1], start=(l == 0), stop=(l == 31))
            O.copy('dve', bias[:], ps[1][:, 0:1])
            for gi in range(2):
                k8 = gi * 4 + kind
                kkeys = [('kT', k8, tc) for tc in range(4)]
                for l in range(32):
                    O.mm(ps[0][:, 0:127], w1[:, l, :], (kT[:, k8, l:l + 16 * 126 + 1:16], kkeys), start=(l == 0), stop=(l == 31))
                O.act(a1[:, 0:127], ps[0][:, 0:127], AF.Silu, bias=bias[:, 0:1])
                if kind == 0:
                    O.mm(ps[2][:, 0:127], w2[:], a1[:, 0:127])
                    O.copy('act', rawb[:, 0:127], ps[2][:, 0:127])
                    O.mm(ps[3][:, 0:127], pm_b[:], rawb[:, 0:127])
                    O.tt('dve', r1[:, 0:127], ps[2][:, 0:127], cosT[:, 0:127], ALU.mult)
                    O.tt('dve', r2[:, 0:127], ps[3][:, 0:127], sinT[:, 0:127], ALU.mult)
                    O.tt('dve', kcc[gi][:, 0:127], r1[:, 0:127], r2[:, 0:127], ALU.add)
                else:
                    O.mm(ps[2][0:127, 0:128], a1[:, 0:127], w2[:])
                    O.copy('dve', Vc[gi][0:127, 0:128], ps[2][0:127, 0:128])
                    O.copy('dve', Vc[gi][:, 128:161], ovl_f[:])
        if 'cmp' in dbg:
            dbg_dump('kcc', [128, 128], kcc[0][:], BF16); dbg_dump('Vc', [128, 168], Vc[0][:], BF16)
        Sd.barrier()
    if stop == 'cmp':
        finish()
        return True

    with ExitStack() as ph:
        maskc_b = sb(ph, 'maskc_b', [128, 1024], BF16); ex_b = sb(ph, 'ex_b', [128, S], BF16)
        tri_b = sb(ph, 'tri_b', [128, 128], BF16); triu_b = sb(ph, 'triu_b', [128, 128], BF16)
        cm = sb(ph, 'cm', [128, 8, 32]); fb = sb(ph, 'fb', [128, 8, 32])
        O.dma('pool', maskc_b[:], IN('maskc')[:, :]); O.dma('pool', ex_b[:], IN('ex')[:, :])
        O.dma('pool', tri_b[:], IN('tri')[:, :]); O.dma('pool', triu_b[:], IN('triu')[:, :])
        O.dma('sp', cm[:], IN('cm')[:, :, :]); O.dma('sp', fb[:], IN('fb')[:, :, :])
        triu_h = sb(ph, 'triu_h', [128, 128], BF16)
        O.ts('dve', triu_h[:], triu_b[:], halo[:, 0:1], None, ALU.mult)
        selT = sb(ph, 'selT', [128, 128], BF16)
        O.memset('dve', selT[:], 0.0)
        Eb = [sb(ph, 'Eb%d' % i, [128, 4, 128], BF16) for i in range(3)]
        Pb = sb(ph, 'Pb', [128, 16, 4, 128], BF16); Pw = sb(ph, 'Pw', [128, 5, 4, 128], BF16)
        msk = [sb(ph, 'msk%d' % i, [128, 128], BF16) for i in range(2)]
        rd = sb(ph, 'rd', [128, 4]); coef = sb(ph, 'coef', [128, 4])
        imp = sb(ph, 'imp', [128, 32]); impf = sb(ph, 'impf', [128, 32]); wk1 = sb(ph, 'wk1', [128, 32]); wk2 = sb(ph, 'wk2', [128, 32])
        m8 = sb(ph, 'm8', [128, 8]); sel = sb(ph, 'sel', [128, 32])
        oacc = sb(ph, 'oacc', [128, 4, 128])
        obuf = [sb(ph, 'obuf%d' % i, [128, 4, 128], BF16) for i in range(2)]
        ecount = [0]

        def acc_view(bank_pair, h, n):
            return bank_pair[h // 2][:, (h % 2) * 256:(h % 2) * 256 + n]

        def den_recip(bank_pair, col, floor=None):
            for bp in range(2):
                v = bank_pair[bp][:, :].rearrange("p (h w) -> p h w", w=256)[:, :, col]
                if floor is not None:
                    O.ts('dve', rd[:, bp * 2:bp * 2 + 2], v, floor, None, ALU.max)
                else:
                    O.copy('dve', rd[:, bp * 2:bp * 2 + 2], v)
            O.recip(rd[:], rd[:])

        for il in range(8):
            i = 8 + il
            for gi in range(2):
                q4 = (qT[:, gi * 4:(gi + 1) * 4, il * 128:(il + 1) * 128], [('qT', gi * 4 + h, il // 4) for h in range(4)])
                accC = (ps[3], ps[4]); accS = (ps[5], ps[6]); accW = accC
                sbank = ps[ecount[0] % 2]; E = Eb[ecount[0] % 3]; ecount[0] += 1
                O.mm(sbank[0:127, :], kcc[gi][:, 0:127], q4)
                O.act(E[0:127, :, :], sbank[0:127, :].rearrange("p (h q) -> p h q", h=4), AF.Exp, scale=SCALE)
                O.tt('pool', E[0:127, :, :], E[0:127, :, :],
                     maskc_b[0:127, il * 128:(il + 1) * 128].unsqueeze(1).to_broadcast([127, 4, 128]), ALU.mult)
                for h in range(4):
                    O.mm(acc_view(accC, h, 161), E[0:127, h, :], Vc[gi][0:127, 0:161])
                den_recip(accC, 160, floor=1e-30)
                O.ts('dve', imp[:], acc_view(accC, 0, 161)[:, 128:160], rd[:, 0:1], None, ALU.mult)
                for h in range(1, 4):
                    O.stt('dve', imp[:], acc_view(accC, h, 161)[:, 128:160], rd[:, h:h + 1], imp[:], ALU.mult, ALU.add)
                O.tt('dve', coef[:], rd[:], (G[:, il, gi * 4:gi * 4 + 4], ('G', il)), ALU.mult)
                for h in range(4):
                    O.ts('dve', oacc[:, h, :], acc_view(accC, h, 161)[:, 0:128], coef[:, h:h + 1], None, ALU.mult)
                O.tt('dve', impf[:], imp[:], cm[:, il, :], ALU.mult)
                O.tt('dve', impf[:], impf[:], fb[:, il, :], ALU.add)
                O.max8(m8[:], impf[:]); O.mrep(wk1[:], m8[:], impf[:], NEGBIG)
                O.max8(m8[:], wk1[:]); O.mrep(wk2[:], m8[:], wk1[:], NEGBIG)
                O.tt('dve', sel[:], impf[:], wk2[:], ALU.not_equal)
                O.tr(ps[2][0:32, 0:128], sel[:, 0:32], ident_f[:])
                O.copy('dve', selT[0:32, :], ps[2][0:32, 0:128])
                if 'sel' in dbg and il == 7 and gi == 0:
                    dbg_dump('sel', [128, 32], sel[:]); dbg_dump('impf', [128, 32], impf[:])
                for kb in range(i + 1):
                    mbank = ps[2]; m_ = msk[kb % 2]
                    O.mm(mbank[:, 128:256], ex_b[:, kb * 128:(kb + 1) * 128], selT[:])
                    if kb == i:
                        O.tt('dve', m_[:], mbank[:, 128:256], tri_b[:], ALU.mult)
                    elif kb < 8:
                        O.ts('dve', m_[:], mbank[:, 128:256], halo[:, 0:1], None, ALU.mult)
                    else:
                        O.copy('dve', m_[:], mbank[:, 128:256])
                    sbank = ps[ecount[0] % 2]; E = Eb[ecount[0] % 3]; ecount[0] += 1
                    O.mm(sbank[:, :], (kT[:, gi * 4 + 1, kb * 128:(kb + 1) * 128], ('kT', gi * 4 + 1, kb // 4)), q4)
                    O.act(E[:], sbank[:, :].rearrange("p (h q) -> p h q", h=4), AF.Exp, scale=SCALE)
                    O.tt('pool', (Pb[:, kb, :, :], ('Pb', kb)), E[:], m_[:].unsqueeze(1).to_broadcast([128, 4, 128]), ALU.mult)
                for h in range(4):
                    for kb in range(i + 1):
                        O.mm(acc_view(accS, h, 129), (Pb[:, kb, h, :], ('Pb', kb)), (Vs[:, kb, gi, 0:129], ('Vs', kb)),
                             start=(kb == 0), stop=(kb == i))
                for wi_, kb in enumerate(range(i - 4, i + 1)):
                    sbank = ps[ecount[0] % 2]; E = Eb[ecount[0] % 3]; ecount[0] += 1
                    O.mm(sbank[:, :], (kT[:, gi * 4 + 2, kb * 128:(kb + 1) * 128], ('kT', gi * 4 + 2, kb // 4)), q4)
                    O.act(E[:], sbank[:, :].rearrange("p (h q) -> p h q", h=4), AF.Exp, scale=SCALE)
                    pw = (Pw[:, wi_, :, :], ('Pw', wi_))
                    if kb == i:
                        O.tt('pool', pw, E[:], tri_b[:].unsqueeze(1).to_broadcast([128, 4, 128]), ALU.mult)
                    elif kb == i - 4:
                        O.tt('pool', pw, E[:], (triu_h if kb < 8 else triu_b)[:].unsqueeze(1).to_broadcast([128, 4, 128]), ALU.mult)
                    elif kb < 8:
                        O.ts('pool', pw, E[:], halo[:, 0:1], None, ALU.mult)
                    else:
                        O.copy('pool', pw, E[:])
                for h in range(4):
                    for wi_, kb in enumerate(range(i - 4, i + 1)):
                        O.mm(acc_view(accW, h, 129), (Pw[:, wi_, h, :], ('Pw', wi_)), (Vw[:, kb, gi, 0:129], ('Vw', kb)),
                             start=(wi_ == 0), stop=(wi_ == 4))
                for (accX, gcol) in ((accS, 8), (accW, 16)):
                    den_recip(accX, 128)
                    O.tt('dve', coef[:], rd[:], (G[:, il, gcol + gi * 4:gcol + gi * 4 + 4], ('G', il)), ALU.mult)
                    for h in range(4):
                        O.stt('dve', oacc[:, h, :], acc_view(accX, h, 129)[:, 0:128], coef[:, h:h + 1], oacc[:, h, :], ALU.mult, ALU.add)
                for h in range(4):
                    O.tr(ps[7][:, h * 128:(h + 1) * 128], oacc[:, h, :], ident_f[:])
                ob_ = obuf[(il * 2 + gi) % 2]
                O.copy('act', ob_[:], ps[7][:, :].rearrange("p (h q) -> p h q", h=4))
                O.dma('sp', (oT_d.ap()[gi * 4:gi * 4 + 4, :, il * 128:(il + 1) * 128].rearrange("h p t -> p h t"), ('oTd', il, gi)), ob_[:])
        if 'attn' in dbg:
            Sd.barrier()
            t_ = C.OUT('dbg_oT', [8, 128, 1024], BF16)
            fin.append(O.dma('sp', t_, oT_d.ap()[0:8]))
        Sd.barrier()
    if stop == 'attn':
        finish()
        return True
    return False


def emit_stage1c(C, Sd, O, es, ps, cfg, ex_out):
    nc = C.nc
    IN = C.IN
    st = ex_out['_state']
    gm_f, sh_f = st['gm_f'], st['sh_f']
    fin, finish, gt_row, norm_T, sb, dbg_dump = (st[k] for k in ('fin', 'finish', 'gt_row', 'norm_T', 'sb', 'dbg_dump'))
    oT_d = ex_out['_oTd']
    Sd.barrier()
    with ExitStack() as ph:
        lt = [sb(ph, 'lt%d' % i, [128, 16, 128], BF16) for i in range(2)]
        wo = sb(ph, 'wo', [128, 16, D], BF16)
        for c4 in range(4):
            O.dma('pool', (wo[:, :, c4 * 512:(c4 + 1) * 512], ('wo', c4)), IN('wo')[:, :, c4 * 512:(c4 + 1) * 512])
        gtm = sb(ph, 'gtm', [128, D]); gt_row(gtm, 32)
        wr = sb(ph, 'wr', [128, 16, 256]); rb = sb(ph, 'rb', [128, 256])
        O.dma('sp', wr[:], IN('wr')[:, :, :]); O.dma('sp', rb[:], IN('rb')[:, :])
        xo = [sb(ph, 'xo%d' % i, [128, D]) for i in range(2)]
        xm = [sb(ph, 'xm%d' % i, [128, D]) for i in range(2)]
        h2f = [sb(ph, 'h2f%d' % i, [128, 16, 128]) for i in range(2)]
        h2b = [sb(ph, 'h2b%d' % i, [128, 16, 128], BF16) for i in range(2)]
        s_ = sb(ph, 'rs_s', [128, 256]); sbb = sb(ph, 'rs_sb', [128, 256]); mk = sb(ph, 'rs_mk', [128, 256])
        selm = sb(ph, 'rs_sel', [128, 256]); gw = sb(ph, 'rs_gw', [128, 256])
        m8g = sb(ph, 'rs_m8g', [128, 8, 8]); gs = sb(ph, 'rs_gs', [128, 8]); gm8 = sb(ph, 'rs_gm8', [128, 8])
        gmask = sb(ph, 'rs_gmask', [128, 8]); tneg = sb(ph, 'rs_tneg', [128, 8]); m8b = sb(ph, 'rs_m8b', [128, 8])
        den = sb(ph, 'rs_den', [128, 1])
        h2T_v = ex_out['h2T'].rearrange("(kc p) t -> p kc t", p=128)

        def load_fn(t):
            O.dma('sp', xo[t % 2][:], IN('xctx')[(8 + t) * 128:(9 + t) * 128, :])
            O.dma('sp', lt[t % 2][:], (oT_d.ap()[:, :, t * 128:(t + 1) * 128].rearrange("k p t -> p k t"), 'oTd_all'))
            for c4 in range(4):
                bank = ps[4 + c4 % 2]
                cs = slice(c4 * 512, (c4 + 1) * 512)
                for kc in range(16):
                    l_ = lt[t % 2][:, kc, :]
                    O.mm(bank[:, :], l_, (wo[:, kc, cs], ('wo', c4)), start=(kc == 0), stop=(kc == 15))
                O.tt('dve', xm[t % 2][:, cs], bank[:, :], gtm[:, cs], ALU.mult)
                O.tt('pool', xm[t % 2][:, cs], xm[t % 2][:, cs], xo[t % 2][:, cs], ALU.add)
            fin.append(O.dma('sp', ex_out['xmid'][t * 128:(t + 1) * 128, :], xm[t % 2][:]))
            return xm[t % 2][:]

        def extra(t, dc, psum_ap, on_act):
            dst2 = (h2b[t % 2][:, dc, :], ('h2b', t, dc))
            if not on_act:
                O.ts('dve', dst2, psum_ap, gm_f[:, dc:dc + 1], sh_f[:, dc:dc + 1], ALU.mult, ALU.add)
            else:
                O.act(dst2, psum_ap, AF.Identity, scale=gm_f[:, dc:dc + 1], bias=sh_f[:, dc:dc + 1])

        def post(t):
            fin.append(O.dma('sp', h2T_v[:, :, t * 128:(t + 1) * 128], (h2b[t % 2][:], [('h2b', t, dc) for dc in range(16)])))
            bank = ps[6]
            for kc in range(16):
                O.mm(bank[:, 0:256], (h2f[t % 2][:, kc, :], ('h2f', t, kc)), wr[:, kc, :], start=(kc == 0), stop=(kc == 15))
            O.act(s_[:], bank[:, 0:256], AF.Sigmoid)
            O.tt('dve', sbb[:], s_[:], rb[:], ALU.add)
            for g8 in range(8):
                O.max8(m8g[:, g8, :], sbb[:, g8 * 32:(g8 + 1) * 32])
            O.tt('dve', gs[:], m8g[:, :, 0], m8g[:, :, 1], ALU.add)
            O.max8(gm8[:], gs[:])
            O.ts('dve', gmask[:], gs[:], gm8[:, 3:4], None, ALU.is_ge)
            O.ts('dve', tneg[:], gmask[:], 1e30, -1e30, ALU.mult, ALU.add)
            O.tt('dve', mk[:].rearrange("p (g e) -> p g e", g=8), sbb[:].rearrange("p (g e) -> p g e", g=8),
                 gmask[:].unsqueeze(2).to_broadcast([128, 8, 32]), ALU.mult)
            O.tt('dve', mk[:].rearrange("p (g e) -> p g e", g=8), mk[:].rearrange("p (g e) -> p g e", g=8),
                 tneg[:].unsqueeze(2).to_broadcast([128, 8, 32]), ALU.add)
            O.max8(m8b[:], mk[:])
            O.ts('dve', selm[:], mk[:], m8b[:, 7:8], None, ALU.is_ge)
            O.tt('dve', selm[:], selm[:], s_[:], ALU.mult)
            O.rsum(den[:], selm[:])
            O.recip(den[:], den[:])
            O.ts('dve', gw[:], selm[:], den[:, 0:1], 2.5, ALU.mult, ALU.mult)
            fin.append(O.dma('sp', ex_out['gw'][t * 128:(t + 1) * 128, :], gw[:]))

        norm_T(ph, load_fn, 8, gm_f, sh_f, lambda t, dc: h2f[t % 2][:, dc, :], 'h2f', extra=extra, pfx='n2', post=post)
        Sd.barrier()
    finish()
    return False


def build_stage1(cfg=None):
    cfg = cfg or {}
    C = Ctx(S1_SPECS)
    nc = C.nc
    ex_out = {'h2T': C.OUT('h2T', [D, 1024], BF16), 'gw': C.OUT('gw', [1024, 256]), 'xmid': C.OUT('xmid', [1024, D]),
              'modT': C.OUT('modT', [96, 128])}
    with ExitStack() as es:
        Sd = Sched(nc, es)
        O = Ops(Sd)
        ps = [es.enter_context(nc.psum_tensor('ps%d' % i, [128, 512], F32)) for i in range(8)]
        emit_stage1(C, Sd, O, es, ps, cfg, ex_out)
        print("stage1 instructions:", Sd.n_inst, flush=True)
    return C


NEXP_LOCAL = 32
ALL_GROUP = [list(range(NCORES))]
S2_SPECS = {
    'goh': ([128, NCORES], F32), 'gfin': ([128, D], F32),
    'weg': ([NEXP_LOCAL, D, 512], F32), 'weu': ([NEXP_LOCAL, D, 512], F32), 'wed': ([NEXP_LOCAL, 512, D], F32),
    'wsg': ([1, D, 512], F32), 'wsu': ([1, D, 512], F32), 'wsd': ([1, 512, D], F32),
}


def emit_convert(C, Sd, O, scr, n_exp):
    for (src, dst, nm, n) in (('wsg', 'wsgb', 'wsgb', 1), ('wsu', 'wsub', 'wsub', 1), ('wsd', 'wsdb', 'wsdb', 1),
                              ('weg', 'wgb', 'wgb', n_exp), ('weu', 'wub', 'wub', n_exp), ('wed', 'wdb', 'wdb', n_exp)):
        pass
    order = []
    for e in range(n_exp):
        order += [('weg', 'wgb', e), ('weu', 'wub', e), ('wed', 'wdb', e)]
        if e == 0:
            order += [('wsg', 'wsgb', 0), ('wsu', 'wsub', 0), ('wsd', 'wsdb', 0)]
    for (src, dst, e) in order:
        s_ap = C.IN(src)[e].rearrange("(a b) n -> a (b n)", a=512)
        t_ap = scr[dst].ap()[e].rearrange("(a b) n -> a (b n)", a=512)
        O.dma('pool', (t_ap, (dst, e)), (s_ap, 'ext_' + src))


def emit_stage2(C, Sd, O, es, ps, cfg, ex, scr):
    nc = C.nc
    IN = C.IN
    n_exp = cfg.get('n_exp', NEXP_LOCAL)
    n_chunks = cfg.get('n_chunks', 16)
    sb = lambda st, name, shape, dt=F32: st.enter_context(nc.sbuf_tensor('t_' + name, list(shape), dt))
    fin = []
    Sd.collective("AllGather", ALU.bypass, [scr['agh_in'].ap().opt()], [scr['agh_out'].ap().opt()], ALL_GROUP,
                  reads=['agh_in'], writes=['agh_out'])
    Sd.collective("AllGather", ALU.bypass, [scr['agw_in'].ap().opt()], [scr['agw_out'].ap().opt()], ALL_GROUP,
                  reads=['agw_in'], writes=['agw_out'])
    with ExitStack() as ph:
        goh = sb(ph, 'goh', [128, NCORES])
        O.dma('sp', goh[:], IN('goh')[:, :])
        h2c = [sb(ph, 'h2c%d' % i, [128, 16, 512], BF16) for i in range(2)]
        wall = [sb(ph, 'wall%d' % i, [128, 4, 256]) for i in range(2)]
        wown = sb(ph, 'wown', [128, 4, 32])
        acc = sb(ph, 'macc', [128, 4, D])
        wg = [sb(ph, 'wg%d' % i, [128, 16, 512], BF16) for i in range(2)]
        wu = [sb(ph, 'wu%d' % i, [128, 16, 512], BF16) for i in range(2)]
        wd = [sb(ph, 'wd%d' % i, [128, 4, D], BF16) for i in range(2)]
        actT = [sb(ph, 'actT%d' % i, [128, 4, 512], BF16) for i in range(2)]
        sg = [sb(ph, 'msg%d' % i, [128, 512]) for i in range(2)]
        cnt = [0]

        def load_weights(i, gname, uname, dname, e):
            O.dma('sp', wg[i][:], (scr[gname].ap()[e].rearrange("(kc p) n -> p kc n", p=128), (gname, e)))
            O.dma('sp', wu[i][:], (scr[uname].ap()[e].rearrange("(kc p) n -> p kc n", p=128), (uname, e)))
            O.dma('sp', wd[i][:], (scr[dname].ap()[e].rearrange("(hc p) n -> p hc n", p=128), (dname, e)))

        def ffn(i, h2, accfn):
            a_ = actT[cnt[0] % 2]
            for hc in range(4):
                bg = ps[(cnt[0] * 2) % 4]; bu = ps[(cnt[0] * 2 + 1) % 4]
                s_ = sg[cnt[0] % 2]
                cnt[0] += 1
                for kc in range(16):
                    O.mm(bg[:, :], wg[i][:, kc, hc * 128:(hc + 1) * 128], h2[:, kc, :], start=(kc == 0), stop=(kc == 15))
                for kc in range(16):
                    O.mm(bu[:, :], wu[i][:, kc, hc * 128:(hc + 1) * 128], h2[:, kc, :], start=(kc == 0), stop=(kc == 15))
                O.act(s_[:], bg[:, :], AF.Silu)
                O.tt('dve', (a_[:, hc, :], (a_.name, hc)), s_[:], bu[:, :], ALU.mult)
            for tt_ in range(4):
                for dch in range(4):
                    bd = ps[4 + (tt_ * 4 + dch) % 4]
                    for hc in range(4):
                        O.mm(bd[:, :], (a_[:, hc, tt_ * 128:(tt_ + 1) * 128], (a_.name, hc)), wd[i][:, hc, dch * 512:(dch + 1) * 512],
                             start=(hc == 0), stop=(hc == 3))
                    accfn(tt_, dch, bd)

        def acc_ap(tt_, dch):
            return (acc[:, tt_, dch * 512:(dch + 1) * 512], ('macc', tt_, dch))
        acc_keys = [('macc', tt_, dch) for tt_ in range(4) for dch in range(4)]
        wcount = [0]

        own_h2 = scr['agh_in'].ap()
        for half in range(2):
            hb = h2c[half % 2]
            O.dma('pool', hb[:], own_h2[:, half * 512:(half + 1) * 512].rearrange("(kc p) t -> p kc t", p=128))
            i = wcount[0] % 2; wcount[0] += 1
            load_weights(i, 'wsgb', 'wsub', 'wsdb', 0)
            ffn(i, hb, lambda tt_, dch, bd: O.copy('dve', acc_ap(tt_, dch), bd[:, :]))
            for tt_ in range(4):
                O.dma('pool', scr['shr_d'].ap()[half * 512 + tt_ * 128:half * 512 + (tt_ + 1) * 128, :],
                      (acc[:, tt_, :], [('macc', tt_, dch) for dch in range(4)]))

        for s in range(n_chunks):
            r_, half = s // 2, s % 2
            hb = h2c[s % 2]; wl = wall[s % 2]
            O.dma('pool', hb[:], scr['agh_out'].ap()[r_ * D:(r_ + 1) * D, half * 512:(half + 1) * 512].rearrange("(kc p) t -> p kc t", p=128))
            O.dma('pool', wl[:], scr['agw_out'].ap()[s * 512:(s + 1) * 512, :].rearrange("(t p) e -> p t e", p=128))
            wv = wl[:].rearrange("p t (g e) -> p t g e", g=NCORES)
            O.ts('dve', wown[:], wv[:, :, 0, :], goh[:, 0:1], None, ALU.mult)
            for g_ in range(1, NCORES):
                O.stt('dve', wown[:], wv[:, :, g_, :], goh[:, g_:g_ + 1], wown[:], ALU.mult, ALU.add)
            for e in range(n_exp):
                i = wcount[0] % 2; wcount[0] += 1
                load_weights(i, 'wgb', 'wub', 'wdb', e)
                if e == 0:
                    fn = lambda tt_, dch, bd, e=e: O.ts('dve', acc_ap(tt_, dch), bd[:, :], wown[:, tt_, e:e + 1], None, ALU.mult)
                else:
                    fn = lambda tt_, dch, bd, e=e: O.stt('dve', acc_ap(tt_, dch), bd[:, :], wown[:, tt_, e:e + 1], acc_ap(tt_, dch), ALU.mult, ALU.add)
                ffn(i, hb, fn)
            for tt_ in range(4):
                O.dma('pool', scr['rs2_in'].ap()[s * 512 + tt_ * 128:s * 512 + (tt_ + 1) * 128, :],
                      (acc[:, tt_, :], [('macc', tt_, dch) for dch in range(4)]))
        Sd.barrier()
    Sd.collective("ReduceScatter", ALU.add, [scr['rs2_in'].ap().opt()], [scr['rs2_out'].ap().opt()], ALL_GROUP,
                  reads=['rs2_in'], writes=['rs2_out'])
    out = ex['out']
    with ExitStack() as ph:
        gfin = sb(ph, 'gfin', [128, D]); gtf = sb(ph, 'gtf', [128, D])
        O.dma('sp', gfin[:], IN('gfin')[:, :])
        modT_d = ex['modT']
        O.dma('sp', gtf[:], (modT_d[80:96, :].rearrange("(o q) p -> o (q p)", o=1).partition_broadcast(128), modT_d.name))
        xm = [sb(ph, 'fxm%d' % i, [128, D]) for i in range(2)]
        rs_ = [sb(ph, 'frs%d' % i, [128, D]) for i in range(2)]
        sh_ = [sb(ph, 'fsh%d' % i, [128, D]) for i in range(2)]
        junk = sb(ph, 'fjunk', [128, D], BF16)
        ssq = sb(ph, 'fssq', [128, 8]); rstd = sb(ph, 'frstd', [128, 8])
        for t in range(8):
            i = t % 2
            rows = slice(t * 128, (t + 1) * 128)
            O.dma('sp', xm[i][:], ex['xmid'][rows, :])
            O.dma('sp', rs_[i][:], scr['rs2_out'].ap()[rows, :])
            O.dma('pool', sh_[i][:], scr['shr_d'].ap()[rows, :])
            O.tt('pool', rs_[i][:], rs_[i][:], sh_[i][:], ALU.add)
            O.tt('dve', rs_[i][:], rs_[i][:], gtf[:], ALU.mult)
            O.tt('pool', xm[i][:], xm[i][:], rs_[i][:], ALU.add)
            kq = ('fssq', t)
            O.act(junk[:], xm[i][:], AF.Square, accum=(ssq[:, t:t + 1], kq))
            O.ts('dve', (rstd[:, t:t + 1], kq), (ssq[:, t:t + 1], kq), 1.0 / D, EPS, ALU.mult, ALU.add)
            O.act((rstd[:, t:t + 1], kq), (rstd[:, t:t + 1], kq), AF.Sqrt)
            O.recip((rstd[:, t:t + 1], kq), (rstd[:, t:t + 1], kq))
            O.stt('dve', sh_[i][:], xm[i][:], (rstd[:, t:t + 1], kq), gfin[:], ALU.mult, ALU.mult)
            fin.append(O.dma('sp', out[rows, :], sh_[i][:]))
        need = {}
        for k, v in fin:
            if need.get(k, 0) < v:
                need[k] = v
        Sd._waits('sp', need)
        Sd.barrier()


def build_fused(cfg=None):
    cfg = cfg or {}
    n_exp = cfg.get('n_exp', NEXP_LOCAL)
    specs = dict(S1_SPECS)
    specs.update(S2_SPECS)
    for k in ('weg', 'weu', 'wed'):
        specs[k] = ([n_exp] + specs[k][0][1:], F32)
    C = Ctx(specs)
    nc = C.nc
    scr = {
        'agh_in': nc.dram_tensor('agh_in', [D, 1024], BF16), 'agh_out': nc.dram_tensor('agh_out', [NCORES * D, 1024], BF16),
        'agw_in': nc.dram_tensor('agw_in', [1024, 256], F32), 'agw_out': nc.dram_tensor('agw_out', [8192, 256], F32),
        'rs2_in': nc.dram_tensor('rs2_in', [8192, D], F32), 'rs2_out': nc.dram_tensor('rs2_out', [1024, D], F32),
        'shr_d': nc.dram_tensor('shr_d', [1024, D], F32), 'xmid_d': nc.dram_tensor('xmid_d', [1024, D], F32),
        'modT_d': nc.dram_tensor('modT_d', [96, 128], F32),
        'wgb': nc.dram_tensor('wgb', [n_exp, D, 512], BF16), 'wub': nc.dram_tensor('wub', [n_exp, D, 512], BF16),
        'wdb': nc.dram_tensor('wdb', [n_exp, 512, D], BF16),
        'wsgb': nc.dram_tensor('wsgb', [1, D, 512], BF16), 'wsub': nc.dram_tensor('wsub', [1, D, 512], BF16),
        'wsdb': nc.dram_tensor('wsdb', [1, 512, D], BF16),
    }
    ex = {'h2T': scr['agh_in'].ap(), 'gw': scr['agw_in'].ap(), 'xmid': scr['xmid_d'].ap(), 'modT': scr['modT_d'].ap(),
          'out': C.OUT('out', [1024, D])}
    with ExitStack() as es:
        Sd = Sched(nc, es)
        O = Ops(Sd)
        ps = [es.enter_context(nc.psum_tensor('ps%d' % i, [128, 512], F32)) for i in range(8)]
        emit_convert(C, Sd, O, scr, n_exp)
        emit_stage1(C, Sd, O, es, ps, cfg, ex)
        emit_stage2(C, Sd, O, es, ps, cfg, ex, scr)
        print("fused instructions:", Sd.n_inst, flush=True)
    return C


def prep_stage2(inp, maps):
    f = lambda k: np.asarray(inp[k])
    gfin = np.ascontiguousarray(np.broadcast_to(f('g_final')[None, :], (128, D))).astype(np.float32)
    for r in range(NCORES):
        m = maps[r]
        goh = np.zeros((128, NCORES), np.float32); goh[:, r] = 1.0
        m['goh'] = goh
        m['gfin'] = gfin
        m['weg'] = f('w_exp_gate')[0][NEXP_LOCAL * r:NEXP_LOCAL * (r + 1)]
        m['weu'] = f('w_exp_up')[0][NEXP_LOCAL * r:NEXP_LOCAL * (r + 1)]
        m['wed'] = f('w_exp_down')[0][NEXP_LOCAL * r:NEXP_LOCAL * (r + 1)]
        m['wsg'] = f('w_sh_gate'); m['wsu'] = f('w_sh_up'); m['wsd'] = f('w_sh_down')
    return maps


_CACHE = {}


def kernel(**inputs):
    if 'prog' not in _CACHE:
        _CACHE['prog'] = build_fused({})
    C = _CACHE['prog']
    maps = prep_stage1(inputs)
    maps = prep_stage2(inputs, maps)
    in_maps = [{k: np.ascontiguousarray(m[k]) for k in C.in_names} for m in maps]
    res = run_bass_kernel_spmd(C.nc, in_maps, core_ids=list(range(NCORES)))
    out = np.zeros((NB, S, D), np.float32)
    for r in range(NCORES):
        b, g = r // 2, r % 2
        out[b, 1024 * g:1024 * (g + 1), :] = np.asarray(res.results[r]['out'])
    return out
```

```python
import numpy as np
from contextlib import ExitStack
import concourse.bass as bass
import concourse.mybir as mybir
from concourse.bass_utils import run_bass_kernel_spmd

F32 = mybir.dt.float32
BF16 = mybir.dt.bfloat16
I32 = mybir.dt.int32
AF = mybir.ActivationFunctionType
ALU = mybir.AluOpType
AX = mybir.AxisListType

D = 2048
S = 2048
NB = 4
NCORES = 8
EPS = 1e-6
SCALE = 128 ** -0.5
PI = float(np.pi)
NEGBIG = -3.0e38


class Sched:
    NDMA = 40
    DMAPOOL = {'sp': (0, 16), 'pool': (16, 16), 'act': (32, 8)}

    def __init__(self, nc, es):
        self.nc = nc
        self.eng = {'pe': nc.tensor, 'act': nc.scalar, 'dve': nc.vector, 'pool': nc.gpsimd, 'sp': nc.sync}
        self.sem = {}
        self.cnt = {}
        for k in ['pe', 'act', 'dve', 'pool', 'cc']:
            self.sem[k] = es.enter_context(nc.semaphore('s_' + k))
            self.cnt[k] = 0
        for i in range(self.NDMA):
            k = 'd%d' % i
            self.sem[k] = es.enter_context(nc.semaphore('s_' + k))
            self.cnt[k] = 0
        self.dma_rr_q = {'sp': 0, 'pool': 0, 'act': 0}
        self.known = {k: {} for k in self.eng}
        self.last_w = {}
        self.readers = {}
        self.n_inst = 0

    def _need(self, reads, writes):
        need = {}

        def add(tok):
            if tok is None:
                return
            k, v = tok
            if need.get(k, 0) < v:
                need[k] = v
        for r in reads:
            add(self.last_w.get(r))
        for w in writes:
            add(self.last_w.get(w))
            for k, v in self.readers.get(w, {}).items():
                add((k, v))
        return need

    def _waits(self, e, need):
        kn = self.known[e]
        for k, v in need.items():
            if k == 'pe' and e == 'pe':
                continue
            if kn.get(k, 0) >= v:
                continue
            self.eng[e].wait_ge(self.sem[k], v)
            kn[k] = v

    def _record(self, tok, reads, writes):
        for r in reads:
            d = self.readers.setdefault(r, {})
            if d.get(tok[0], 0) < tok[1]:
                d[tok[0]] = tok[1]
        for w in writes:
            self.last_w[w] = tok
            self.readers[w] = {}

    def op(self, e, fn, reads=(), writes=()):
        pr = [k for k in reads if isinstance(k, str) and k.startswith('ps') and k not in writes]
        if pr:
            writes = list(writes) + pr
        self._waits(e, self._need(reads, writes))
        ins = fn(self.eng[e])
        ins.then_inc(self.sem[e], 1)
        self.cnt[e] += 1
        tok = (e, self.cnt[e])
        self._record(tok, reads, writes)
        self.n_inst += 1
        return tok

    def dma(self, q, out, in_, reads=(), writes=(), **kw):
        self._waits(q, self._need(reads, writes))
        lo, n = self.DMAPOOL[q]
        i = lo + self.dma_rr_q[q]
        self.dma_rr_q[q] = (self.dma_rr_q[q] + 1) % n
        k = 'd%d' % i
        if self.cnt[k] > 0 and self.known[q].get(k, 0) < self.cnt[k]:
            self.eng[q].wait_ge(self.sem[k], self.cnt[k])
            self.known[q][k] = self.cnt[k]
        ins = self.eng[q].dma_start(out=out, in_=in_, **kw)
        ins.then_inc(self.sem[k], 16)
        self.cnt[k] += 16
        tok = (k, self.cnt[k])
        self._record(tok, reads, writes)
        self.n_inst += 1
        return tok

    def collective(self, kind, alu, ins, outs, groups, reads=(), writes=()):
        self._waits('pool', self._need(reads, writes))
        ins_ = self.nc.gpsimd.collective_compute(kind, alu, replica_groups=groups, ins=ins, outs=outs)
        ins_.then_inc(self.sem['cc'])
        self.cnt['cc'] += 1
        tok = ('cc', self.cnt['cc'])
        self._record(tok, reads, writes)
        return tok

    def barrier(self):
        need = {k: v for k, v in self.cnt.items() if v > 0}
        for e in self.eng:
            kn = self.known[e]
            for k, v in need.items():
                if kn.get(k, 0) >= v:
                    continue
                self.eng[e].wait_ge(self.sem[k], v)
                kn[k] = v


def _pk(w, n=None):
    K, N = w.shape
    return np.ascontiguousarray(w.reshape(K // 128, 128, N).transpose(1, 0, 2))


def _consts(g):
    c = {}
    c['ident'] = np.eye(128, dtype=np.float32)
    r = np.arange(128)
    c['tri'] = (r[:, None] <= r[None, :]).astype(np.float32)
    c['triu'] = (r[:, None] > r[None, :]).astype(np.float32)
    pm = np.zeros((128, 128), np.float32)
    for dd in range(64):
        pm[dd + 64, dd] = -1.0
        pm[dd, dd + 64] = 1.0
    c['pm'] = pm
    off = 1024 * (1 - g)
    j = np.arange(1024)
    tctx = 1024 + j
    cc = np.arange(127)
    mk = ((cc[:, None] * 16 + 31) <= tctx[None, :]) & ((cc[:, None] * 16) >= off)
    c['maskc'] = np.zeros((128, 1024), np.float32)
    c['maskc'][:127] = mk.astype(np.float32)
    cs = cc * 16
    js = np.arange(32) * 64
    ovl = ((cs[:, None] < js[None, :] + 64) & (cs[:, None] + 32 > js[None, :])).astype(np.float32)
    o33 = np.zeros((128, 33), np.float32)
    o33[:127, :32] = ovl
    o33[:127, 32] = 1.0
    c['ovl'] = o33
    jbc = np.arange(32)
    jbt = jbc - off // 64
    ttrue = tctx - off
    cur = (ttrue // 64)[:, None]
    exist = (jbt >= 0)[None, :]
    forced = exist & ((jbt[None, :] == 0) | (jbt[None, :] == cur) | (jbt[None, :] == cur - 1))
    causal = exist & (jbt[None, :] * 64 <= ttrue[:, None])
    cm = (causal & ~forced).astype(np.float32)
    fb = np.where(forced, 1e30, np.where(causal, 0.0, -1e30)).astype(np.float32)
    c['cm'] = np.ascontiguousarray(cm.reshape(8, 128, 32).transpose(1, 0, 2))
    c['fb'] = np.ascontiguousarray(fb.reshape(8, 128, 32).transpose(1, 0, 2))
    ex = np.zeros((128, S), np.float32)
    ex[:32] = (np.arange(S)[None, :] // 64 == jbc[:, None]).astype(np.float32)
    c['ex'] = ex
    half = 64
    inv = (10000.0 ** (-np.arange(half, dtype=np.float32) / half)).astype(np.float32)
    c['invf'] = np.concatenate([inv, inv]).reshape(128, 1).astype(np.float32)
    c['halo'] = np.full((128, 1), float(g), np.float32)
    return c


def prep_stage1(inp):
    f = lambda k: np.asarray(inp[k])
    x = f('x'); c = f('c'); pos = f('positions')
    w_in = f('w_in')[0]
    vec16 = lambda v: np.ascontiguousarray(v.reshape(16, 128).T).astype(np.float32)
    vec8 = lambda v: np.ascontiguousarray(v.reshape(8, 128).T).astype(np.float32)
    sh = {}
    sh['wada'] = _pk(f('w_ada')[0])
    sh['bada'] = np.ascontiguousarray(f('b_ada')[0].reshape(96, 128).T)
    sh['gmix'] = vec16(f('g_mix')[0]); sh['gffn'] = vec16(f('g_ffn')[0])
    for nm, key in (('k', 'cmp_k'), ('v', 'cmp_v')):
        sh['c%sw1' % nm] = np.ascontiguousarray(f(key + '_w1')[0].reshape(32, 128, 128).transpose(1, 0, 2))
        sh['c%sw2' % nm] = np.ascontiguousarray(f(key + '_w2')[0])
        sh['c%spe' % nm] = np.ascontiguousarray(f(key + '_pe')[0].T)
    sh['convw'] = np.ascontiguousarray(f('conv_w')[0][:, 0, :].reshape(31, 8, 128).transpose(2, 1, 0))
    sh['convb'] = vec8(f('conv_b')[0]); sh['lng'] = vec8(f('conv_ln_g')[0]); sh['lnb'] = vec8(f('conv_ln_b')[0])
    sh['wq'] = _pk(w_in[:, 0:1024])
    kcols = []
    for gi in range(2):
        for base in (1024, 1536, 2048, 1280):
            kcols.append(np.arange(base + 128 * gi, base + 128 * gi + 128))
    sh['wkt'] = _pk(w_in[:, np.concatenate(kcols)])
    vcols = [np.arange(1792, 1792 + 256), np.arange(2304, 2304 + 256), np.arange(2560, 2584)]
    sh['wvn'] = _pk(w_in[:, np.concatenate(vcols)])
    sh['wglu'] = _pk(w_in[:, 2584:2584 + 2048])
    sh['wo'] = _pk(f('w_out')[0])
    sh['wr'] = _pk(f('w_router')[0])
    sh['rb'] = np.ascontiguousarray(np.broadcast_to(f('router_bias')[0][None, :], (128, 256))).astype(np.float32)
    cons = [_consts(0), _consts(1)]
    maps = []
    for r in range(NCORES):
        b, g = r // 2, r % 2
        m = dict(sh)
        m.update(cons[g])
        m['cTb'] = np.ascontiguousarray(c[b].reshape(16, 128).T).astype(np.float32)
        xc = np.zeros((S, D), np.float32)
        pc = np.zeros((S,), np.int32)
        if g == 1:
            xc[:] = x[b]; pc[:] = pos[b]
        else:
            xc[1024:] = x[b, :1024]; pc[1024:] = pos[b, :1024]
        m['xctx'] = xc
        m['posr'] = np.ascontiguousarray(np.broadcast_to(pc[None, :], (128, S))).astype(np.int32)
        maps.append(m)
    return maps


def _ak(x):
    if isinstance(x, tuple):
        ap, k = x
        return ap, (list(k) if isinstance(k, list) else [k])
    return x, [x.name]


class Ops:
    def __init__(self, S_):
        self.S = S_

    def act(self, out, in_, func, bias=None, scale=None, accum=None, eng='act'):
        o, ok = _ak(out); i, ik = _ak(in_)
        rk = list(ik); wk = list(ok); kw = {}
        if bias is not None:
            if isinstance(bias, (float, int)):
                kw['bias'] = float(bias)
            else:
                b, bk = _ak(bias); kw['bias'] = b; rk += bk
        if scale is not None:
            if isinstance(scale, (float, int)):
                kw['scale'] = float(scale)
            else:
                s, sk = _ak(scale); kw['scale'] = s; rk += sk
        if accum is not None:
            a, akk = _ak(accum); kw['accum_out'] = a; wk += akk
        return self.S.op('act', lambda e: e.activation(out=o, in_=i, func=func, **kw), rk, wk)

    def tt(self, eng, out, in0, in1, op):
        o, ok = _ak(out); a, ak = _ak(in0); b, bk = _ak(in1)
        return self.S.op(eng, lambda e: e.tensor_tensor(out=o, in0=a, in1=b, op=op), ak + bk, ok)

    def ts(self, eng, out, in0, s1, s2, op0, op1=None, accum=None):
        o, ok = _ak(out); a, ak = _ak(in0)
        rk = list(ak); wk = list(ok)
        if not isinstance(s1, (float, int)):
            s1, k1 = _ak(s1); rk += k1
        if s2 is not None and not isinstance(s2, (float, int)):
            s2, k2 = _ak(s2); rk += k2
        kw = {}
        if op1 is not None:
            kw['op1'] = op1
        if accum is not None:
            ac, ack = _ak(accum); kw['accum_out'] = ac; wk += ack
        return self.S.op(eng, lambda e: e.tensor_scalar(out=o, in0=a, scalar1=s1, scalar2=s2, op0=op0, **kw), rk, wk)

    def stt(self, eng, out, in0, scalar, in1, op0, op1):
        o, ok = _ak(out); a, ak = _ak(in0); b, bk = _ak(in1)
        rk = ak + bk
        if not isinstance(scalar, (float, int)):
            scalar, sk = _ak(scalar); rk = rk + sk
        return self.S.op(eng, lambda e: e.scalar_tensor_tensor(out=o, in0=a, scalar=scalar, in1=b, op0=op0, op1=op1), rk, ok)

    def copy(self, eng, out, in_):
        o, ok = _ak(out); i, ik = _ak(in_)
        if eng == 'act':
            return self.S.op('act', lambda e: e.activation(out=o, in_=i, func=AF.Copy), ik, ok)
        return self.S.op(eng, lambda e: e.tensor_copy(out=o, in_=i), ik, ok)

    def memset(self, eng, out, val):
        o, ok = _ak(out)
        return self.S.op(eng, lambda e: e.memset(o, val), [], ok)

    def recip(self, out, in_):
        o, ok = _ak(out); i, ik = _ak(in_)
        return self.S.op('dve', lambda e: e.reciprocal(out=o, in_=i), ik, ok)

    def mm(self, out, lhsT, rhs, start=True, stop=True):
        o, ok = _ak(out); l, lk = _ak(lhsT); r, rk = _ak(rhs)
        return self.S.op('pe', lambda e: e.matmul(o, lhsT=l, rhs=r, start=start, stop=stop), lk + rk, ok)

    def tr(self, out, in_, ident):
        o, ok = _ak(out); i, ik = _ak(in_); d, dk = _ak(ident)
        return self.S.op('pe', lambda e: e.transpose(out=o, in_=i, identity=d), ik + dk, ok)

    def rsum(self, out, in_):
        o, ok = _ak(out); i, ik = _ak(in_)
        return self.S.op('dve', lambda e: e.reduce_sum(out=o, in_=i, axis=AX.X), ik, ok)

    def max8(self, out, in_):
        o, ok = _ak(out); i, ik = _ak(in_)
        return self.S.op('dve', lambda e: e.max(out=o, in_=i), ik, ok)

    def mrep(self, out, rep, vals, imm):
        o, ok = _ak(out); r, rk = _ak(rep); v, vk = _ak(vals)
        return self.S.op('dve', lambda e: e.match_replace(out=o, in_to_replace=r, in_values=v, imm_value=imm), rk + vk, ok)

    def dma(self, q, out, in_, **kw):
        o, ok = _ak(out); i, ik = _ak(in_)
        return self.S.dma(q, o, i, reads=ik, writes=ok, **kw)


class Ctx:
    def __init__(self, specs):
        self.nc = bass.Bass("TRN2", target_bir_lowering=False)
        self.specs = specs
        self.in_names = []
        self._decl = {}
        self.outs = {}

    def IN(self, name):
        if name not in self._decl:
            shp, dt = self.specs[name]
            self.in_names.append(name)
            self._decl[name] = self.nc.dram_tensor(name, list(shp), dt, kind="ExternalInput").ap()
        return self._decl[name]

    def OUT(self, name, shape, dt=F32):
        self.outs[name] = self.nc.dram_tensor(name, list(shape), dt, kind="ExternalOutput").ap()
        return self.outs[name]


S1_SPECS = {
    'ident': ([128, 128], F32), 'tri': ([128, 128], F32), 'triu': ([128, 128], F32), 'pm': ([128, 128], F32),
    'maskc': ([128, 1024], F32), 'ovl': ([128, 33], F32), 'cm': ([128, 8, 32], F32), 'fb': ([128, 8, 32], F32),
    'ex': ([128, S], F32), 'invf': ([128, 1], F32), 'halo': ([128, 1], F32),
    'cTb': ([128, 16], F32), 'wada': ([128, 16, 12288], F32), 'bada': ([128, 96], F32),
    'gmix': ([128, 16], F32), 'gffn': ([128, 16], F32),
    'ckw1': ([128, 32, 128], F32), 'ckw2': ([128, 128], F32), 'ckpe': ([128, 32], F32),
    'cvw1': ([128, 32, 128], F32), 'cvw2': ([128, 128], F32), 'cvpe': ([128, 32], F32),
    'convw': ([128, 8, 31], F32), 'convb': ([128, 8], F32), 'lng': ([128, 8], F32), 'lnb': ([128, 8], F32),
    'wq': ([128, 16, 1024], F32), 'wkt': ([128, 16, 1024], F32), 'wvn': ([128, 16, 536], F32),
    'wglu': ([128, 16, 2048], F32), 'wo': ([128, 16, D], F32), 'wr': ([128, 16, 256], F32), 'rb': ([128, 256], F32),
    'xctx': ([S, D], F32), 'posr': ([128, S], I32),
}


def emit_stage1(C, Sd, O, es, ps, cfg, ex_out):
    nc = C.nc
    IN = C.IN
    dbg = set(cfg.get('dbg', ()))
    stop = cfg.get('stop', 'end')
    fin = []
    sb = lambda st, name, shape, dt=F32: st.enter_context(nc.sbuf_tensor('t_' + name, list(shape), dt))

    def dbg_dump(name, shape, src_ap, dt=F32):
        Sd.barrier()
        t_ = C.OUT('dbg_' + name, shape, dt)
        fin.append(O.dma('sp', t_, src_ap))

    ident_f = sb(es, 'ident_f', [128, 128]); ident_b = sb(es, 'ident_b', [128, 128], BF16)
    modown = sb(es, 'modown', [128, 96]); gm_m = sb(es, 'gm_m', [128, 16]); gm_f = sb(es, 'gm_f', [128, 16])
    halo = sb(es, 'halo', [128, 1])
    O.dma('sp', ident_f[:], IN('ident')[:, :])
    O.copy('dve', ident_b[:], ident_f[:])
    O.dma('sp', halo[:], IN('halo')[:, :])
    modT_d = ex_out['modT']

    with ExitStack() as ph:
        wsl = [sb(ph, 'wsl%d' % i, [128, 16, 768]) for i in range(2)]
        cTb = sb(ph, 'cTb', [128, 16]); scT = sb(ph, 'scT', [128, 16])
        bada = sb(ph, 'bada', [128, 96]); gmix = sb(ph, 'gmix', [128, 16]); gffn = sb(ph, 'gffn', [128, 16])
        modT = sb(ph, 'modT', [96, 128])
        O.dma('sp', cTb[:], IN('cTb')[:, :]); O.dma('sp', bada[:], IN('bada')[:, :])
        O.dma('sp', gmix[:], IN('gmix')[:, :]); O.dma('sp', gffn[:], IN('gffn')[:, :])
        O.act(scT[:], cTb[:], AF.Silu)
        for sl in range(16):
            w_ = wsl[sl % 2]
            O.dma('sp' if sl % 2 == 0 else 'act', w_[:], IN('wada')[:, :, sl * 768:(sl + 1) * 768])
            for jj in range(6):
                q = sl * 6 + jj
                for kc in range(16):
                    O.mm(ps[0][:, q:q + 1], w_[:, kc, jj * 128:(jj + 1) * 128], scT[:, kc:kc + 1],
                         start=(kc == 0), stop=(kc == 15))
        O.tt('dve', modown[:], ps[0][:, 0:96], bada[:], ALU.add)
        O.stt('dve', gm_m[:], modown[:, 16:32], 1.0, gmix[:], ALU.add, ALU.mult)
        O.stt('dve', gm_f[:], modown[:, 64:80], 1.0, gffn[:], ALU.add, ALU.mult)
        O.tr(ps[1][0:96, 0:128], modown[:, 0:96], ident_f[:])
        O.copy('dve', modT[:], ps[1][0:96, 0:128])
        O.dma('sp', modT_d, modT[:])
        if 'mod' in dbg:
            dbg_dump('mod', [128, 96], modown[:])
        Sd.barrier()
    sh_m = modown[:, 0:16]; sh_f = modown[:, 48:64]

    def finish():
        need = {}
        for k, v in fin:
            if need.get(k, 0) < v:
                need[k] = v
        Sd._waits('sp', need)
        Sd.barrier()

    if stop == 'A':
        finish()
        return True

    def gt_row(dst, q0):
        O.dma('sp', dst[:], (modT_d[q0:q0 + 16, :].rearrange("(o q) p -> o (q p)", o=1).partition_broadcast(128), modT_d.name))

    def norm_T(ph, load_fn, ntiles, gm, shv, dstT, dkey, extra=None, pfx='', post=None):
        xn = [sb(ph, pfx + 'xn%d' % i, [128, D]) for i in range(2)]
        junk = sb(ph, pfx + 'junk', [128, D], BF16)
        ssq = sb(ph, pfx + 'ssq', [128, ntiles]); rs = sb(ph, pfx + 'rs', [128, ntiles])
        for t in range(ntiles):
            xt = load_fn(t)
            xnt = xn[t % 2]
            kq = (pfx + 'ssq', t); kr = (pfx + 'rs', t)
            O.act(junk[:], xt, AF.Square, accum=(ssq[:, t:t + 1], kq))
            O.ts('dve', (rs[:, t:t + 1], kr), (ssq[:, t:t + 1], kq), 1.0 / D, EPS, ALU.mult, ALU.add)
            O.act((rs[:, t:t + 1], kr), (rs[:, t:t + 1], kr), AF.Sqrt)
            O.recip((rs[:, t:t + 1], kr), (rs[:, t:t + 1], kr))
            O.act(xnt[:], xt, AF.Copy, scale=(rs[:, t:t + 1], kr))
            for g4 in range(4):
                bank = ps[(t * 4 + g4) % 4]
                for j in range(4):
                    dc = g4 * 4 + j
                    O.tr(bank[:, j * 128:(j + 1) * 128], xnt[:, dc * 128:(dc + 1) * 128], ident_f[:])
                for j in range(4):
                    dc = g4 * 4 + j
                    dst = (dstT(t, dc), (dkey, t, dc))
                    if g4 % 2 == 0:
                        O.act(dst, bank[:, j * 128:(j + 1) * 128], AF.Identity, scale=gm[:, dc:dc + 1], bias=shv[:, dc:dc + 1])
                    else:
                        O.ts('dve', dst, bank[:, j * 128:(j + 1) * 128], gm[:, dc:dc + 1], shv[:, dc:dc + 1], ALU.mult, ALU.add)
                    if extra is not None:
                        extra(t, dc, bank[:, j * 128:(j + 1) * 128], g4 % 2 == 0)
            if post is not None:
                post(t)

    oT_d = nc.dram_tensor('oT_d', [16, 128, 1024], BF16)
    ex_out['_oTd'] = oT_d
    stopped = [False]
    with ExitStack() as pm_:
        qT = sb(pm_, 'qT', [128, 8, 1024], BF16)
        kT = sb(pm_, 'kT', [128, 8, S], BF16)
        Vs = sb(pm_, 'Vs', [128, 16, 2, 132], BF16); Vw = sb(pm_, 'Vw', [128, 16, 2, 132], BF16)
        G = sb(pm_, 'G', [128, 8, 24])
        pm_b = sb(pm_, 'pm_b', [128, 128], BF16)
        y = sb(pm_, 'y', [128, 8, 1152], BF16)
        cosC = sb(pm_, 'cosC', [128, 128], BF16); sinC = sb(pm_, 'sinC', [128, 128], BF16)
        pt_ = pm_.enter_context(ExitStack())
        cosT = sb(pt_, 'cosT', [128, S], BF16); sinT = sb(pt_, 'sinT', [128, S], BF16)
        O.memset('pool', Vs[:, :, :, 128:132], 1.0); O.memset('pool', Vw[:, :, :, 128:132], 1.0)
        with ExitStack() as ph:
            tmp = sb(ph, 'pmtmp', [128, 128])
            O.dma('sp', tmp[:], IN('pm')[:, :]); O.copy('dve', pm_b[:], tmp[:])
            posi = sb(ph, 'posi', [128, S], I32); ang = sb(ph, 'ang', [128, S]); invf = sb(ph, 'invf', [128, 1])
            ta = sb(ph, 'ta', [128, S]); tb = sb(ph, 'tb', [128, S]); ki = sb(ph, 'ki', [128, S], I32)
            O.dma('sp', posi[:], IN('posr')[:, :]); O.dma('sp', invf[:], IN('invf')[:, :])
            O.copy('dve', ang[:], posi[:])
            O.ts('dve', ang[:], ang[:], invf[:, 0:1], None, ALU.mult)
            for (shift, table) in ((0.0, sinT), (PI / 2, cosT)):
                O.ts('dve', ta[:], ang[:], shift, 1.0 / (2 * PI), ALU.add, ALU.mult)
                O.copy('dve', ki[:], ta[:]); O.copy('dve', tb[:], ki[:])
                O.ts('dve', ta[:], ta[:], 2 * PI, None, ALU.mult)
                O.stt('dve', ta[:], tb[:], -2 * PI, ta[:], ALU.mult, ALU.add)
                O.ts('dve', tb[:], ta[:], PI, None, ALU.is_gt)
                O.stt('dve', ta[:], tb[:], -2 * PI, ta[:], ALU.mult, ALU.add)
                O.ts('dve', tb[:], ta[:], -PI, None, ALU.is_lt)
                O.stt('dve', ta[:], tb[:], 2 * PI, ta[:], ALU.mult, ALU.add)
                O.ts('dve', ta[:], ta[:], PI, -PI, ALU.min, ALU.max)
                O.act(table[:], ta[:], AF.Sin)
            csl_ = slice(31, 31 + 16 * 126 + 1, 16)
            O.copy('dve', cosC[:, 0:127], cosT[:, csl_]); O.copy('dve', sinC[:, 0:127], sinT[:, csl_])
            if 'tab' in dbg:
                dbg_dump('cosT', [128, S], cosT[:], BF16); dbg_dump('sinT', [128, S], sinT[:], BF16)
            Sd.barrier()
        if stop == 'tab':
            finish()
            return True

        with ExitStack() as ph:
            hT = sb(ph, 'hT', [128, 16, S], BF16)
            with ExitStack() as ph2:
                xbuf = [sb(ph2, 'xbuf%d' % i, [128, D]) for i in range(2)]

                def load_ctx(t):
                    O.dma('sp', xbuf[t % 2][:], IN('xctx')[t * 128:(t + 1) * 128, :])
                    return xbuf[t % 2][:]
                norm_T(ph2, load_ctx, 16, gm_m, sh_m, lambda t, dc: hT[:, dc, t * 128:(t + 1) * 128], 'hT')
                Sd.barrier()
            if 'hT' in dbg:
                t_ = C.OUT('dbg_hT', [128, 16, S], BF16)
                for dc in range(16):
                    fin.append(O.dma('sp', t_[:, dc, :], (hT[:, dc, :], [('hT', t, dc) for t in range(16)])))
            if stop == 'norm':
                finish()
                return True
            hk = lambda t0, t1: [('hT', t, dc) for t in range(t0, t1) for dc in range(16)]
            rawb0 = sb(ph, 'rawb0', [128, 512], BF16); r10 = sb(ph, 'r1_0', [128, 512]); r20 = sb(ph, 'r2_0', [128, 512])
            rawb = [rawb0, rawb0]; r1 = [r10, r10]; r2 = [r20, r20]
            wb1 = sb(ph, 'wb0', [128, 16, 512], BF16)
            wb = [wb1, wb1]
            cnt = [0]

            def rope_evac(raw_ps, sw_ps, dst, c0, n, tcos, tsin):
                i = cnt[0] % 2; cnt[0] += 1
                O.copy('act', rawb[i][:, 0:n], raw_ps)
                O.mm(sw_ps, pm_b[:], rawb[i][:, 0:n])
                O.tt('dve', r1[i][:, 0:n], raw_ps, tcos, ALU.mult)
                O.tt('dve', r2[i][:, 0:n], sw_ps, tsin, ALU.mult)
                O.tt('pool', dst, r1[i][:, 0:n], r2[i][:, 0:n], ALU.add)

            def load_w(i, name, c0, n):
                O.dma('pool', wb[i][:, :, 0:n], IN(name)[:, :, c0:c0 + n])
            wi = 0
            for half in range(2):
                load_w(wi % 2, 'wq', half * 512, 512)
                for hh in range(4):
                    h8 = half * 4 + hh
                    for tc in range(2):
                        bank = ps[4 + (h8 * 2 + tc) % 2]; bank2 = ps[6 + (h8 * 2 + tc) % 2]
                        t0 = 8 + tc * 4
                        for kc in range(16):
                            O.mm(bank[:, :], wb[wi % 2][:, kc, hh * 128:(hh + 1) * 128],
                                 (hT[:, kc, t0 * 128:(t0 + 4) * 128], [('hT', t, kc) for t in range(t0, t0 + 4)]),
                                 start=(kc == 0), stop=(kc == 15))
                        if 'q0' in dbg and h8 == 0 and tc == 0:
                            dq = sb(ph, 'dq', [128, 512])
                            O.copy('dve', dq[:], bank[:, :])
                            dbg_dump('q0raw', [128, 512], dq[:])
                            t_ = C.OUT('dbg_wb', [128, 16, 512], BF16)
                            for kc_ in range(16):
                                fin.append(O.dma('sp', t_[:, kc_, :], wb[0][:, kc_, :]))
                        rope_evac(bank[:, :], bank2[:, :], (qT[:, h8, tc * 512:(tc + 1) * 512], ('qT', h8, tc)), 0, 512,
                                  cosT[:, t0 * 128:(t0 + 4) * 128], sinT[:, t0 * 128:(t0 + 4) * 128])
                wi += 1
            if stop == 'q':
                dbg_dump('qT', [128, 8, 1024], qT[:], BF16)
                finish()
                return True
            for half in range(2):
                load_w(wi % 2, 'wkt', half * 512, 512)
                for kk in range(4):
                    k8 = half * 4 + kk
                    kind = k8 % 4
                    for tc in range(4):
                        bank = ps[4 + (k8 * 4 + tc) % 2]; bank2 = ps[6 + (k8 * 4 + tc) % 2]
                        t0 = tc * 4
                        for kc in range(16):
                            O.mm(bank[:, :], wb[wi % 2][:, kc, kk * 128:(kk + 1) * 128],
                                 (hT[:, kc, t0 * 128:(t0 + 4) * 128], [('hT', t, kc) for t in range(t0, t0 + 4)]),
                                 start=(kc == 0), stop=(kc == 15))
                        dst = (kT[:, k8, tc * 512:(tc + 1) * 512], ('kT', k8, tc))
                        if kind in (1, 2):
                            rope_evac(bank[:, :], bank2[:, :], dst, 0, 512,
                                      cosT[:, t0 * 128:(t0 + 4) * 128], sinT[:, t0 * 128:(t0 + 4) * 128])
                        else:
                            O.copy('act', dst, bank[:, :])
                wi += 1
            for (c0, n) in ((0, 512), (512, 24)):
                load_w(wi % 2, 'wvn', c0, n)
                for t in range(16):
                    if n == 24 and t < 8:
                        continue
                    bank = ps[4 + t % 2]
                    for kc in range(16):
                        O.mm(bank[:, 0:n], (hT[:, kc, t * 128:(t + 1) * 128], ('hT', t, kc)), wb[wi % 2][:, kc, 0:n],
                             start=(kc == 0), stop=(kc == 15))
                    if n == 512:
                        O.copy('act', (Vs[:, t, :, 0:128], ('Vs', t)), bank[:, 0:256].rearrange("p (g d) -> p g d", g=2))
                        O.copy('dve', (Vw[:, t, :, 0:128], ('Vw', t)), bank[:, 256:512].rearrange("p (g d) -> p g d", g=2))
                    else:
                        O.act((G[:, t - 8, :], ('G', t - 8)), bank[:, 0:24], AF.Sigmoid)
                wi += 1
            if 'proj' in dbg:
                dbg_dump('qT', [128, 8, 1024], qT[:], BF16)
                dbg_dump('kT', [128, 8, S], kT[:], BF16)
                dbg_dump('Vs', [128, 16, 2, 132], Vs[:], BF16)
                dbg_dump('G', [128, 8, 24], G[:])
            if stop == 'proj':
                finish()
                return True

            sg0 = sb(ph, 'sg0', [128, 512], BF16)
            sg = [sg0, sg0]
            TCH = ((896, 512), (1408, 512), (1920, 128))
            for cch in range(8):
                O.dma('pool', wb[wi % 2][:, :, 0:128], IN('wglu')[:, :, cch * 128:(cch + 1) * 128])
                O.dma('pool', wb[wi % 2][:, :, 128:256], IN('wglu')[:, :, 1024 + cch * 128:1024 + (cch + 1) * 128])
                for ti, (c0, n) in enumerate(TCH):
                    ba = ps[4 + (cch * 3 + ti) % 2]; bb = ps[6 + (cch * 3 + ti) % 2]
                    keys = lambda kc: [('hT', t, kc) for t in range(c0 // 128, (c0 + n) // 128)]
                    for kc in range(16):
                        O.mm(ba[:, 0:n], wb[wi % 2][:, kc, 0:128], (hT[:, kc, c0:c0 + n], keys(kc)), start=(kc == 0), stop=(kc == 15))
                    for kc in range(16):
                        O.mm(bb[:, 0:n], wb[wi % 2][:, kc, 128:256], (hT[:, kc, c0:c0 + n], keys(kc)), start=(kc == 0), stop=(kc == 15))
                    s_ = sg[(cch * 3 + ti) % 2]
                    O.act(s_[:, 0:n], bb[:, 0:n], AF.Sigmoid)
                    O.tt('dve', (y[:, cch, c0 - 896:c0 - 896 + n], ('y', cch, ti)), ba[:, 0:n], s_[:, 0:n], ALU.mult)
                O.ts('pool', (y[:, cch, 0:128], ('y', cch, 0)), (y[:, cch, 0:128], ('y', cch, 0)), halo[:, 0:1], None, ALU.mult)
                wi += 1
            Sd.barrier()
        pt_.close()
        ex_out['_state'] = dict(qT=qT, kT=kT, Vs=Vs, Vw=Vw, G=G, cosT=cosC, sinT=sinC, y=y, pm_b=pm_b,
                                ident_f=ident_f, ident_b=ident_b, halo=halo, modown=modown, gm_f=gm_f, sh_f=sh_f,
                                fin=fin, finish=finish, gt_row=gt_row, norm_T=norm_T, sb=sb, dbg_dump=dbg_dump, pm_=pm_)
        stopped[0] = emit_stage1b(C, Sd, O, es, ps, cfg, ex_out)
    if stopped[0]:
        return True
    return emit_stage1c(C, Sd, O, es, ps, cfg, ex_out)


def emit_stage1b(C, Sd, O, es, ps, cfg, ex_out):
    nc = C.nc
    IN = C.IN
    st = ex_out['_state']
    qT, kT, Vs, Vw, G, cosT, sinT, y, pm_b = (st[k] for k in ('qT', 'kT', 'Vs', 'Vw', 'G', 'cosT', 'sinT', 'y', 'pm_b'))
    ident_f, ident_b, halo, modown, gm_f, sh_f = (st[k] for k in ('ident_f', 'ident_b', 'halo', 'modown', 'gm_f', 'sh_f'))
    fin, finish, gt_row, norm_T, sb, dbg_dump, pm_ = (st[k] for k in ('fin', 'finish', 'gt_row', 'norm_T', 'sb', 'dbg_dump', 'pm_'))
    dbg = set(cfg.get('dbg', ()))
    stop = cfg.get('stop', 'end')
    oT_d = ex_out['_oTd']

    with ExitStack() as ph:
        convw = sb(ph, 'convw', [128, 8, 31]); convb = sb(ph, 'convb', [128, 8])
        lng = sb(ph, 'lng', [128, 8]); lnb = sb(ph, 'lnb', [128, 8])
        acc = sb(ph, 'cacc', [128, 8, 1024]); ones_f = sb(ph, 'ones_f', [128, 128])
        sq = [sb(ph, 'sq%d' % i, [128, 1024]) for i in range(2)]
        mean = sb(ph, 'mean', [128, 1024]); rstd = sb(ph, 'rstd', [128, 1024])
        oct_ = [sb(ph, 'oct%d' % i, [128, 1024], BF16) for i in range(2)]
        O.dma('sp', convw[:], IN('convw')[:, :, :]); O.dma('sp', convb[:], IN('convb')[:, :])
        O.dma('sp', lng[:], IN('lng')[:, :]); O.dma('sp', lnb[:], IN('lnb')[:, :])
        O.memset('dve', ones_f[:], 1.0)
        for cch in range(8):
            e = 'dve'
            ykeys = [('y', cch, ti) for ti in range(3)]
            a_c = (acc[:, cch, :], ('cacc', cch))
            O.ts(e, a_c, (y[:, cch, 98:98 + 1024], ykeys), convw[:, cch, 0:1], convb[:, cch:cch + 1], ALU.mult, ALU.add)
            for k in range(1, 31):
                O.stt(e, a_c, (y[:, cch, 98 + k:98 + k + 1024], ykeys), convw[:, cch, k:k + 1], a_c, ALU.mult, ALU.add)
        for cch in range(8):
            a_c = (acc[:, cch, :], ('cacc', cch))
            s_ = sq[cch % 2]
            O.act(s_[:], a_c, AF.Square)
            for tc in range(2):
                O.mm(ps[tc][:, :], ones_f[:], (acc[:, cch, tc * 512:(tc + 1) * 512], ('cacc', cch)), start=(cch == 0), stop=(cch == 7))
                O.mm(ps[2 + tc][:, :], ones_f[:], s_[:, tc * 512:(tc + 1) * 512], start=(cch == 0), stop=(cch == 7))
        for tc in range(2):
            sl = slice(tc * 512, (tc + 1) * 512)
            O.act(mean[:, sl], ps[tc][:, :], AF.Copy, scale=1.0 / 1024)
            O.tt('dve', rstd[:, sl], mean[:, sl], mean[:, sl], ALU.mult)
            O.stt('dve', rstd[:, sl], ps[2 + tc][:, :], 1.0 / 1024, rstd[:, sl], ALU.mult, ALU.subtract)
            O.ts('dve', rstd[:, sl], rstd[:, sl], EPS, None, ALU.add)
            O.act(rstd[:, sl], rstd[:, sl], AF.Sqrt)
            O.recip(rstd[:, sl], rstd[:, sl])
        for cch in range(8):
            e = 'dve' if cch % 2 == 0 else 'pool'
            a_c = (acc[:, cch, :], ('cacc', cch))
            O.tt(e, a_c, a_c, mean[:], ALU.subtract)
            O.tt(e, a_c, a_c, rstd[:], ALU.mult)
            oc_t = oct_[cch % 2]
            O.act(oc_t[:], a_c, AF.Silu, scale=lng[:, cch:cch + 1], bias=lnb[:, cch:cch + 1])
            O.dma('sp', (oT_d.ap()[8 + cch], ('oTd', 8 + cch)), oc_t[:])
        if 'conv' in dbg:
            Sd.barrier()
            t_ = C.OUT('dbg_ocT', [8, 128, 1024], BF16)
            fin.append(O.dma('sp', t_, oT_d.ap()[8:16]))
        Sd.barrier()
    if stop == 'conv':
        finish()
        return True

    kcc = [sb(pm_, 'kcc%d' % gi, [128, 128], BF16) for gi in range(2)]
    Vc = [sb(pm_, 'Vc%d' % gi, [128, 168], BF16) for gi in range(2)]
    with ExitStack() as ph:
        w1 = sb(ph, 'cw1', [128, 32, 128], BF16); w2 = sb(ph, 'cw2', [128, 128], BF16); pe = sb(ph, 'cpe', [128, 32], BF16)
        bias = sb(ph, 'cbias', [128, 1]); a1 = sb(ph, 'ca1', [128, 128], BF16)
        ovl_f = sb(ph, 'ovl_f', [128, 33])
        rawb = sb(ph, 'crawb', [128, 128], BF16); r1 = sb(ph, 'cr1', [128, 128]); r2 = sb(ph, 'cr2', [128, 128])
        O.dma('sp', ovl_f[:], IN('ovl')[:, :])
        for gi in range(2):
            O.memset('dve', Vc[gi][:], 0.0); O.memset('dve', kcc[gi][:], 0.0)
        csl = slice(31, 31 + 16 * 126 + 1, 16)
        for kind, nm in ((0, 'k'), (3, 'v')):
            O.dma('pool', w1[:], IN('c%sw1' % nm)[:, :, :]); O.dma('pool', w2[:], IN('c%sw2' % nm)[:, :])
            O.dma('pool', pe[:], IN('c%spe' % nm)[:, :])
            for l in range(32):
                O.mm(ps[1][:, 0:1], w1[:, l, :], pe[:, l:l + 1], start=(l == 0), stop=(l == 31))
            O.copy('dve', bias[:], ps[1][:, 0:1])
            for gi in range(2):
                k8 = gi * 4 + kind
                kkeys = [('kT', k8, tc) for tc in range(4)]
                for l in range(32):
                    O.mm(ps[0][:, 0:127], w1[:, l, :], (kT[:, k8, l:l + 16 * 126 + 1:16], kkeys), start=(l == 0), stop=(l == 31))
                O.act(a1[:, 0:127], ps[0][:, 0:127], AF.Silu, bias=bias[:, 0:1])
                if kind == 0:
                    O.mm(ps[2][:, 0:127], w2[:], a1[:, 0:127])
                    O.copy('act', rawb[:, 0:127], ps[2][:, 0:127])
                    O.mm(ps[3][:, 0:127], pm_b[:], rawb[:, 0:127])
                    O.tt('dve', r1[:, 0:127], ps[2][:, 0:127], cosT[:, 0:127], ALU.mult)
                    O.tt('dve', r2[:, 0:127], ps[3][:, 0:127], sinT[:, 0:127], ALU.mult)
                    O.tt('dve', kcc[gi][:, 0:127], r1[:, 0:127], r2[:, 0:127], ALU.add)
                else:
                    O.mm(ps[2][0:127, 0:128], a1[:, 0:127], w2[:])
                    O.copy('dve', Vc[gi][0:127, 0:128], ps[2][0:127, 0:128])
                    O.copy('dve', Vc[gi][:, 128:161], ovl_f[:])
        if 'cmp' in dbg:
            dbg_dump('kcc', [128, 128], kcc[0][:], BF16); dbg_dump('Vc', [128, 168], Vc[0][:], BF16)
        Sd.barrier()
    if stop == 'cmp':
        finish()
        return True

    with ExitStack() as ph:
        maskc_b = sb(ph, 'maskc_b', [128, 1024], BF16); ex_b = sb(ph, 'ex_b', [128, S], BF16)
        tri_b = sb(ph, 'tri_b', [128, 128], BF16); triu_b = sb(ph, 'triu_b', [128, 128], BF16)
        cm = sb(ph, 'cm', [128, 8, 32]); fb = sb(ph, 'fb', [128, 8, 32])
        O.dma('pool', maskc_b[:], IN('maskc')[:, :]); O.dma('pool', ex_b[:], IN('ex')[:, :])
        O.dma('pool', tri_b[:], IN('tri')[:, :]); O.dma('pool', triu_b[:], IN('triu')[:, :])
        O.dma('sp', cm[:], IN('cm')[:, :, :]); O.dma('sp', fb[:], IN('fb')[:, :, :])
        triu_h = sb(ph, 'triu_h', [128, 128], BF16)
        O.ts('dve', triu_h[:], triu_b[:], halo[:, 0:1], None, ALU.mult)
        selT = sb(ph, 'selT', [128, 128], BF16)
        O.memset('dve', selT[:], 0.0)
        Eb = [sb(ph, 'Eb%d' % i, [128, 4, 128], BF16) for i in range(3)]
        Pb = sb(ph, 'Pb', [128, 16, 4, 128], BF16); Pw = sb(ph, 'Pw', [128, 5, 4, 128], BF16)
        msk = [sb(ph, 'msk%d' % i, [128, 128], BF16) for i in range(2)]
        rd = sb(ph, 'rd', [128, 4]); coef = sb(ph, 'coef', [128, 4])
        imp = sb(ph, 'imp', [128, 32]); impf = sb(ph, 'impf', [128, 32]); wk1 = sb(ph, 'wk1', [128, 32]); wk2 = sb(ph, 'wk2', [128, 32])
        m8 = sb(ph, 'm8', [128, 8]); sel = sb(ph, 'sel', [128, 32])
        oacc = sb(ph, 'oacc', [128, 4, 128])
        obuf = [sb(ph, 'obuf%d' % i, [128, 4, 128], BF16) for i in range(2)]
        ecount = [0]

        def acc_view(bank_pair, h, n):
            return bank_pair[h // 2][:, (h % 2) * 256:(h % 2) * 256 + n]

        def den_recip(bank_pair, col, floor=None):
            for bp in range(2):
                v = bank_pair[bp][:, :].rearrange("p (h w) -> p h w", w=256)[:, :, col]
                if floor is not None:
                    O.ts('dve', rd[:, bp * 2:bp * 2 + 2], v, floor, None, ALU.max)
                else:
                    O.copy('dve', rd[:, bp * 2:bp * 2 + 2], v)
            O.recip(rd[:], rd[:])

        for il in range(8):
            i = 8 + il
            for gi in range(2):
                q4 = (qT[:, gi * 4:(gi + 1) * 4, il * 128:(il + 1) * 128], [('qT', gi * 4 + h, il // 4) for h in range(4)])
                accC = (ps[3], ps[4]); accS = (ps[5], ps[6]); accW = accC
                sbank = ps[ecount[0] % 2]; E = Eb[ecount[0] % 3]; ecount[0] += 1
                O.mm(sbank[0:127, :], kcc[gi][:, 0:127], q4)
                O.act(E[0:127, :, :], sbank[0:127, :].rearrange("p (h q) -> p h q", h=4), AF.Exp, scale=SCALE)
                O.tt('pool', E[0:127, :, :], E[0:127, :, :],
                     maskc_b[0:127, il * 128:(il + 1) * 128].unsqueeze(1).to_broadcast([127, 4, 128]), ALU.mult)
                for h in range(4):
                    O.mm(acc_view(accC, h, 161), E[0:127, h, :], Vc[gi][0:127, 0:161])
                den_recip(accC, 160, floor=1e-30)
                O.ts('dve', imp[:], acc_view(accC, 0, 161)[:, 128:160], rd[:, 0:1], None, ALU.mult)
                for h in range(1, 4):
                    O.stt('dve', imp[:], acc_view(accC, h, 161)[:, 128:160], rd[:, h:h + 1], imp[:], ALU.mult, ALU.add)
                O.tt('dve', coef[:], rd[:], (G[:, il, gi * 4:gi * 4 + 4], ('G', il)), ALU.mult)
                for h in range(4):
                    O.ts('dve', oacc[:, h, :], acc_view(accC, h, 161)[:, 0:128], coef[:, h:h + 1], None, ALU.mult)
                O.tt('dve', impf[:], imp[:], cm[:, il, :], ALU.mult)
                O.tt('dve', impf[:], impf[:], fb[:, il, :], ALU.add)
                O.max8(m8[:], impf[:]); O.mrep(wk1[:], m8[:], impf[:], NEGBIG)
                O.max8(m8[:], wk1[:]); O.mrep(wk2[:], m8[:], wk1[:], NEGBIG)
                O.tt('dve', sel[:], impf[:], wk2[:], ALU.not_equal)
                O.tr(ps[2][0:32, 0:128], sel[:, 0:32], ident_f[:])
                O.copy('dve', selT[0:32, :], ps[2][0:32, 0:128])
                if 'sel' in dbg and il == 7 and gi == 0:
                    dbg_dump('sel', [128, 32], sel[:]); dbg_dump('impf', [128, 32], impf[:])
                for kb in range(i + 1):
                    mbank = ps[2]; m_ = msk[kb % 2]
                    O.mm(mbank[:, 128:256], ex_b[:, kb * 128:(kb + 1) * 128], selT[:])
                    if kb == i:
                        O.tt('dve', m_[:], mbank[:, 128:256], tri_b[:], ALU.mult)
                    elif kb < 8:
                        O.ts('dve', m_[:], mbank[:, 128:256], halo[:, 0:1], None, ALU.mult)
                    else:
                        O.copy('dve', m_[:], mbank[:, 128:256])
                    sbank = ps[ecount[0] % 2]; E = Eb[ecount[0] % 3]; ecount[0] += 1
                    O.mm(sbank[:, :], (kT[:, gi * 4 + 1, kb * 128:(kb + 1) * 128], ('kT', gi * 4 + 1, kb // 4)), q4)
                    O.act(E[:], sbank[:, :].rearrange("p (h q) -> p h q", h=4), AF.Exp, scale=SCALE)
                    O.tt('pool', (Pb[:, kb, :, :], ('Pb', kb)), E[:], m_[:].unsqueeze(1).to_broadcast([128, 4, 128]), ALU.mult)
                for h in range(4):
                    for kb in range(i + 1):
                        O.mm(acc_view(accS, h, 129), (Pb[:, kb, h, :], ('Pb', kb)), (Vs[:, kb, gi, 0:129], ('Vs', kb)),
                             start=(kb == 0), stop=(kb == i))
                for wi_, kb in enumerate(range(i - 4, i + 1)):
                    sbank = ps[ecount[0] % 2]; E = Eb[ecount[0] % 3]; ecount[0] += 1
                    O.mm(sbank[:, :], (kT[:, gi * 4 + 2, kb * 128:(kb + 1) * 128], ('kT', gi * 4 + 2, kb // 4)), q4)
                    O.act(E[:], sbank[:, :].rearrange("p (h q) -> p h q", h=4), AF.Exp, scale=SCALE)
                    pw = (Pw[:, wi_, :, :], ('Pw', wi_))
                    if kb == i:
                        O.tt('pool', pw, E[:], tri_b[:].unsqueeze(1).to_broadcast([128, 4, 128]), ALU.mult)
                    elif kb == i - 4:
                        O.tt('pool', pw, E[:], (triu_h if kb < 8 else triu_b)[:].unsqueeze(1).to_broadcast([128, 4, 128]), ALU.mult)
                    elif kb < 8:
                        O.ts('pool', pw, E[:], halo[:, 0:1], None, ALU.mult)
                    else:
                        O.copy('pool', pw, E[:])
                for h in range(4):
                    for wi_, kb in enumerate(range(i - 4, i + 1)):
                        O.mm(acc_view(accW, h, 129), (Pw[:, wi_, h, :], ('Pw', wi_)), (Vw[:, kb, gi, 0:129], ('Vw', kb)),
                             start=(wi_ == 0), stop=(wi_ == 4))
                for (accX, gcol) in ((accS, 8), (accW, 16)):
                    den_recip(accX, 128)
                    O.tt('dve', coef[:], rd[:], (G[:, il, gcol + gi * 4:gcol + gi * 4 + 4], ('G', il)), ALU.mult)
                    for h in range(4):
                        O.stt('dve', oacc[:, h, :], acc_view(accX, h, 129)[:, 0:128], coef[:, h:h + 1], oacc[:, h, :], ALU.mult, ALU.add)
                for h in range(4):
                    O.tr(ps[7][:, h * 128:(h + 1) * 128], oacc[:, h, :], ident_f[:])
                ob_ = obuf[(il * 2 + gi) % 2]
                O.copy('act', ob_[:], ps[7][:, :].rearrange("p (h q) -> p h q", h=4))
                O.dma('sp', (oT_d.ap()[gi * 4:gi * 4 + 4, :, il * 128:(il + 1) * 128].rearrange("h p t -> p h t"), ('oTd', il, gi)), ob_[:])
        if 'attn' in dbg:
            Sd.barrier()
            t_ = C.OUT('dbg_oT', [8, 128, 1024], BF16)
            fin.append(O.dma('sp', t_, oT_d.ap()[0:8]))
        Sd.barrier()
    if stop == 'attn':
        finish()
        return True
    return False


def emit_stage1c(C, Sd, O, es, ps, cfg, ex_out):
    nc = C.nc
    IN = C.IN
    st = ex_out['_state']
    gm_f, sh_f = st['gm_f'], st['sh_f']
    fin, finish, gt_row, norm_T, sb, dbg_dump = (st[k] for k in ('fin', 'finish', 'gt_row', 'norm_T', 'sb', 'dbg_dump'))
    oT_d = ex_out['_oTd']
    Sd.barrier()
    with ExitStack() as ph:
        lt = [sb(ph, 'lt%d' % i, [128, 16, 128], BF16) for i in range(2)]
        wo = sb(ph, 'wo', [128, 16, D], BF16)
        for c4 in range(4):
            O.dma('pool', (wo[:, :, c4 * 512:(c4 + 1) * 512], ('wo', c4)), IN('wo')[:, :, c4 * 512:(c4 + 1) * 512])
        gtm = sb(ph, 'gtm', [128, D]); gt_row(gtm, 32)
        wr = sb(ph, 'wr', [128, 16, 256]); rb = sb(ph, 'rb', [128, 256])
        O.dma('sp', wr[:], IN('wr')[:, :, :]); O.dma('sp', rb[:], IN('rb')[:, :])
        xo = [sb(ph, 'xo%d' % i, [128, D]) for i in range(2)]
        xm = [sb(ph, 'xm%d' % i, [128, D]) for i in range(2)]
        h2f = [sb(ph, 'h2f%d' % i, [128, 16, 128]) for i in range(2)]
        h2b = [sb(ph, 'h2b%d' % i, [128, 16, 128], BF16) for i in range(2)]
        s_ = sb(ph, 'rs_s', [128, 256]); sbb = sb(ph, 'rs_sb', [128, 256]); mk = sb(ph, 'rs_mk', [128, 256])
        selm = sb(ph, 'rs_sel', [128, 256]); gw = sb(ph, 'rs_gw', [128, 256])
        m8g = sb(ph, 'rs_m8g', [128, 8, 8]); gs = sb(ph, 'rs_gs', [128, 8]); gm8 = sb(ph, 'rs_gm8', [128, 8])
        gmask = sb(ph, 'rs_gmask', [128, 8]); tneg = sb(ph, 'rs_tneg', [128, 8]); m8b = sb(ph, 'rs_m8b', [128, 8])
        den = sb(ph, 'rs_den', [128, 1])
        h2T_v = ex_out['h2T'].rearrange("(kc p) t -> p kc t", p=128)

        def load_fn(t):
            O.dma('sp', xo[t % 2][:], IN('xctx')[(8 + t) * 128:(9 + t) * 128, :])
            O.dma('sp', lt[t % 2][:], (oT_d.ap()[:, :, t * 128:(t + 1) * 128].rearrange("k p t -> p k t"), 'oTd_all'))
            for c4 in range(4):
                bank = ps[4 + c4 % 2]
                cs = slice(c4 * 512, (c4 + 1) * 512)
                for kc in range(16):
                    l_ = lt[t % 2][:, kc, :]
                    O.mm(bank[:, :], l_, (wo[:, kc, cs], ('wo', c4)), start=(kc == 0), stop=(kc == 15))
                O.tt('dve', xm[t % 2][:, cs], bank[:, :], gtm[:, cs], ALU.mult)
                O.tt('pool', xm[t % 2][:, cs], xm[t % 2][:, cs], xo[t % 2][:, cs], ALU.add)
            fin.append(O.dma('sp', ex_out['xmid'][t * 128:(t + 1) * 128, :], xm[t % 2][:]))
            return xm[t % 2][:]

        def extra(t, dc, psum_ap, on_act):
            dst2 = (h2b[t % 2][:, dc, :], ('h2b', t, dc))
            if not on_act:
                O.ts('dve', dst2, psum_ap, gm_f[:, dc:dc + 1], sh_f[:, dc:dc + 1], ALU.mult, ALU.add)
            else:
                O.act(dst2, psum_ap, AF.Identity, scale=gm_f[:, dc:dc + 1], bias=sh_f[:, dc:dc + 1])

        def post(t):
            fin.append(O.dma('sp', h2T_v[:, :, t * 128:(t + 1) * 128], (h2b[t % 2][:], [('h2b', t, dc) for dc in range(16)])))
            bank = ps[6]
            for kc in range(16):
                O.mm(bank[:, 0:256], (h2f[t % 2][:, kc, :], ('h2f', t, kc)), wr[:, kc, :], start=(kc == 0), stop=(kc == 15))
            O.act(s_[:], bank[:, 0:256], AF.Sigmoid)
            O.tt('dve', sbb[:], s_[:], rb[:], ALU.add)
            for g8 in range(8):
                O.max8(m8g[:, g8, :], sbb[:, g8 * 32:(g8 + 1) * 32])
            O.tt('dve', gs[:], m8g[:, :, 0], m8g[:, :, 1], ALU.add)
            O.max8(gm8[:], gs[:])
            O.ts('dve', gmask[:], gs[:], gm8[:, 3:4], None, ALU.is_ge)
            O.ts('dve', tneg[:], gmask[:], 1e30, -1e30, ALU.mult, ALU.add)
            O.tt('dve', mk[:].rearrange("p (g e) -> p g e", g=8), sbb[:].rearrange("p (g e) -> p g e", g=8),
                 gmask[:].unsqueeze(2).to_broadcast([128, 8, 32]), ALU.mult)
            O.tt('dve', mk[:].rearrange("p (g e) -> p g e", g=8), mk[:].rearrange("p (g e) -> p g e", g=8),
                 tneg[:].unsqueeze(2).to_broadcast([128, 8, 32]), ALU.add)
            O.max8(m8b[:], mk[:])
            O.ts('dve', selm[:], mk[:], m8b[:, 7:8], None, ALU.is_ge)
            O.tt('dve', selm[:], selm[:], s_[:], ALU.mult)
            O.rsum(den[:], selm[:])
            O.recip(den[:], den[:])
            O.ts('dve', gw[:], selm[:], den[:, 0:1], 2.5, ALU.mult, ALU.mult)
            fin.append(O.dma('sp', ex_out['gw'][t * 128:(t + 1) * 128, :], gw[:]))

        norm_T(ph, load_fn, 8, gm_f, sh_f, lambda t, dc: h2f[t % 2][:, dc, :], 'h2f', extra=extra, pfx='n2', post=post)
        Sd.barrier()
    finish()
    return False


def build_stage1(cfg=None):
    cfg = cfg or {}
    C = Ctx(S1_SPECS)
    nc = C.nc
    ex_out = {'h2T': C.OUT('h2T', [D, 1024], BF16), 'gw': C.OUT('gw', [1024, 256]), 'xmid': C.OUT('xmid', [1024, D]),
              'modT': C.OUT('modT', [96, 128])}
    with ExitStack() as es:
        Sd = Sched(nc, es)
        O = Ops(Sd)
        ps = [es.enter_context(nc.psum_tensor('ps%d' % i, [128, 512], F32)) for i in range(8)]
        emit_stage1(C, Sd, O, es, ps, cfg, ex_out)
        print("stage1 instructions:", Sd.n_inst, flush=True)
    return C


NEXP_LOCAL = 32
ALL_GROUP = [list(range(NCORES))]
S2_SPECS = {
    'goh': ([128, NCORES], F32), 'gfin': ([128, D], F32),
    'weg': ([NEXP_LOCAL, D, 512], F32), 'weu': ([NEXP_LOCAL, D, 512], F32), 'wed': ([NEXP_LOCAL, 512, D], F32),
    'wsg': ([1, D, 512], F32), 'wsu': ([1, D, 512], F32), 'wsd': ([1, 512, D], F32),
}


def emit_convert(C, Sd, O, scr, n_exp):
    for (src, dst, nm, n) in (('wsg', 'wsgb', 'wsgb', 1), ('wsu', 'wsub', 'wsub', 1), ('wsd', 'wsdb', 'wsdb', 1),
                              ('weg', 'wgb', 'wgb', n_exp), ('weu', 'wub', 'wub', n_exp), ('wed', 'wdb', 'wdb', n_exp)):
        pass
    order = []
    for e in range(n_exp):
        order += [('weg', 'wgb', e), ('weu', 'wub', e), ('wed', 'wdb', e)]
        if e == 0:
            order += [('wsg', 'wsgb', 0), ('wsu', 'wsub', 0), ('wsd', 'wsdb', 0)]
    for (src, dst, e) in order:
        s_ap = C.IN(src)[e].rearrange("(a b) n -> a (b n)", a=512)
        t_ap = scr[dst].ap()[e].rearrange("(a b) n -> a (b n)", a=512)
        O.dma('pool', (t_ap, (dst, e)), (s_ap, 'ext_' + src))


def emit_stage2(C, Sd, O, es, ps, cfg, ex, scr):
    nc = C.nc
    IN = C.IN
    n_exp = cfg.get('n_exp', NEXP_LOCAL)
    n_chunks = cfg.get('n_chunks', 16)
    sb = lambda st, name, shape, dt=F32: st.enter_context(nc.sbuf_tensor('t_' + name, list(shape), dt))
    fin = []
    multi = cfg.get('multi', False)
    if not multi:
        Sd.collective("AllGather", ALU.bypass, [scr['agh_in'].ap().opt()], [scr['agh_out'].ap().opt()], ALL_GROUP,
                      reads=['agh_in'], writes=['agh_out'])
        Sd.collective("AllGather", ALU.bypass, [scr['agw_in'].ap().opt()], [scr['agw_out'].ap().opt()], ALL_GROUP,
                      reads=['agw_in'], writes=['agw_out'])
    with ExitStack() as ph:
        goh = sb(ph, 'goh', [128, NCORES])
        O.dma('sp', goh[:], IN('goh')[:, :])
        h2c = [sb(ph, 'h2c%d' % i, [128, 16, 512], BF16) for i in range(2)]
        wall = [sb(ph, 'wall%d' % i, [128, 4, 256]) for i in range(2)]
        wown = sb(ph, 'wown', [128, 4, 32])
        acc = sb(ph, 'macc', [128, 4, D])
        wg = [sb(ph, 'wg%d' % i, [128, 16, 512], BF16) for i in range(2)]
        wu = [sb(ph, 'wu%d' % i, [128, 16, 512], BF16) for i in range(2)]
        wd = [sb(ph, 'wd%d' % i, [128, 4, D], BF16) for i in range(2)]
        actT = [sb(ph, 'actT%d' % i, [128, 4, 512], BF16) for i in range(2)]
        sg = [sb(ph, 'msg%d' % i, [128, 512]) for i in range(2)]
        cnt = [0]

        def load_weights(i, gname, uname, dname, e):
            O.dma('sp', wg[i][:], (scr[gname].ap()[e].rearrange("(kc p) n -> p kc n", p=128), (gname, e)))
            O.dma('sp', wu[i][:], (scr[uname].ap()[e].rearrange("(kc p) n -> p kc n", p=128), (uname, e)))
            O.dma('sp', wd[i][:], (scr[dname].ap()[e].rearrange("(hc p) n -> p hc n", p=128), (dname, e)))

        def ffn(i, h2, accfn):
            a_ = actT[cnt[0] % 2]
            for hc in range(4):
                bg = ps[(cnt[0] * 2) % 4]; bu = ps[(cnt[0] * 2 + 1) % 4]
                s_ = sg[cnt[0] % 2]
                cnt[0] += 1
                for kc in range(16):
                    O.mm(bg[:, :], wg[i][:, kc, hc * 128:(hc + 1) * 128], h2[:, kc, :], start=(kc == 0), stop=(kc == 15))
                for kc in range(16):
                    O.mm(bu[:, :], wu[i][:, kc, hc * 128:(hc + 1) * 128], h2[:, kc, :], start=(kc == 0), stop=(kc == 15))
                O.act(s_[:], bg[:, :], AF.Silu)
                O.tt('dve', (a_[:, hc, :], (a_.name, hc)), s_[:], bu[:, :], ALU.mult)
            for tt_ in range(4):
                for dch in range(4):
                    bd = ps[4 + (tt_ * 4 + dch) % 4]
                    for hc in range(4):
                        O.mm(bd[:, :], (a_[:, hc, tt_ * 128:(tt_ + 1) * 128], (a_.name, hc)), wd[i][:, hc, dch * 512:(dch + 1) * 512],
                             start=(hc == 0), stop=(hc == 3))
                    accfn(tt_, dch, bd)

        def acc_ap(tt_, dch):
            return (acc[:, tt_, dch * 512:(dch + 1) * 512], ('macc', tt_, dch))
        acc_keys = [('macc', tt_, dch) for tt_ in range(4) for dch in range(4)]
        wcount = [0]

        own_h2 = scr['agh_in'].ap()
        for half in range(2):
            hb = h2c[half % 2]
            O.dma('pool', hb[:], own_h2[:, half * 512:(half + 1) * 512].rearrange("(kc p) t -> p kc t", p=128))
            i = wcount[0] % 2; wcount[0] += 1
            load_weights(i, 'wsgb', 'wsub', 'wsdb', 0)
            ffn(i, hb, lambda tt_, dch, bd: O.copy('dve', acc_ap(tt_, dch), bd[:, :]))
            for tt_ in range(4):
                fin.append(O.dma('pool', scr['shr_d'].ap()[half * 512 + tt_ * 128:half * 512 + (tt_ + 1) * 128, :],
                                 (acc[:, tt_, :], [('macc', tt_, dch) for dch in range(4)])))

        for s in range(n_chunks):
            r_, half = s // 2, s % 2
            hb = h2c[s % 2]; wl = wall[s % 2]
            O.dma('pool', hb[:], scr['agh_out'].ap()[r_ * D:(r_ + 1) * D, half * 512:(half + 1) * 512].rearrange("(kc p) t -> p kc t", p=128))
            O.dma('pool', wl[:], scr['agw_out'].ap()[s * 512:(s + 1) * 512, :].rearrange("(t p) e -> p t e", p=128))
            wv = wl[:].rearrange("p t (g e) -> p t g e", g=NCORES)
            O.ts('dve', wown[:], wv[:, :, 0, :], goh[:, 0:1], None, ALU.mult)
            for g_ in range(1, NCORES):
                O.stt('dve', wown[:], wv[:, :, g_, :], goh[:, g_:g_ + 1], wown[:], ALU.mult, ALU.add)
            for e in range(n_exp):
                i = wcount[0] % 2; wcount[0] += 1
                load_weights(i, 'wgb', 'wub', 'wdb', e)
                if e == 0:
                    fn = lambda tt_, dch, bd, e=e: O.ts('dve', acc_ap(tt_, dch), bd[:, :], wown[:, tt_, e:e + 1], None, ALU.mult)
                else:
                    fn = lambda tt_, dch, bd, e=e: O.stt('dve', acc_ap(tt_, dch), bd[:, :], wown[:, tt_, e:e + 1], acc_ap(tt_, dch), ALU.mult, ALU.add)
                ffn(i, hb, fn)
            for tt_ in range(4):
                fin.append(O.dma('pool', scr['rs2_in'].ap()[s * 512 + tt_ * 128:s * 512 + (tt_ + 1) * 128, :],
                                 (acc[:, tt_, :], [('macc', tt_, dch) for dch in range(4)])))
        Sd.barrier()
    if multi:
        need = {}
        for k, v in fin:
            if need.get(k, 0) < v:
                need[k] = v
        Sd._waits('sp', need)
        Sd.barrier()
        return
    Sd.collective("ReduceScatter", ALU.add, [scr['rs2_in'].ap().opt()], [scr['rs2_out'].ap().opt()], ALL_GROUP,
                  reads=['rs2_in'], writes=['rs2_out'])
    emit_final(C, Sd, O, es, cfg, ex, scr, None)


def emit_final(C, Sd, O, es, cfg, ex, scr, parts):
    nc = C.nc
    IN = C.IN
    sb = lambda st, name, shape, dt=F32: st.enter_context(nc.sbuf_tensor('t_' + name, list(shape), dt))
    fin = []
    out = ex['out']
    with ExitStack() as ph:
        gfin = sb(ph, 'gfin', [128, D]); gtf = sb(ph, 'gtf', [128, D])
        O.dma('sp', gfin[:], IN('gfin')[:, :])
        modT_d = ex['modT']
        O.dma('sp', gtf[:], (modT_d[80:96, :].rearrange("(o q) p -> o (q p)", o=1).partition_broadcast(128), modT_d.name))
        xm = [sb(ph, 'fxm%d' % i, [128, D]) for i in range(2)]
        rs_ = [sb(ph, 'frs%d' % i, [128, D]) for i in range(2)]
        sh_ = [sb(ph, 'fsh%d' % i, [128, D]) for i in range(2)]
        junk = sb(ph, 'fjunk', [128, D], BF16)
        pbuf = [sb(ph, 'fpb%d' % i, [128, D]) for i in range(2)]
        ssq = sb(ph, 'fssq', [128, 8]); rstd = sb(ph, 'frstd', [128, 8])
        for t in range(8):
            i = t % 2
            rows = slice(t * 128, (t + 1) * 128)
            O.dma('sp', xm[i][:], ex['xmid'][rows, :])
            if parts is None:
                O.dma('sp', rs_[i][:], scr['rs2_out'].ap()[rows, :])
            else:
                O.dma('sp', rs_[i][:], parts[0, rows, :])
                for g_ in range(1, NCORES):
                    pb = pbuf[g_ % 2]
                    O.dma('sp' if g_ % 2 == 0 else 'act', pb[:], parts[g_, rows, :])
                    O.tt('dve' if g_ % 2 == 0 else 'pool', rs_[i][:], rs_[i][:], pb[:], ALU.add)
            O.dma('pool', sh_[i][:], scr['shr_d'].ap()[rows, :])
            O.tt('pool', rs_[i][:], rs_[i][:], sh_[i][:], ALU.add)
            O.tt('dve', rs_[i][:], rs_[i][:], gtf[:], ALU.mult)
            O.tt('pool', xm[i][:], xm[i][:], rs_[i][:], ALU.add)
            kq = ('fssq', t)
            O.act(junk[:], xm[i][:], AF.Square, accum=(ssq[:, t:t + 1], kq))
            O.ts('dve', (rstd[:, t:t + 1], kq), (ssq[:, t:t + 1], kq), 1.0 / D, EPS, ALU.mult, ALU.add)
            O.act((rstd[:, t:t + 1], kq), (rstd[:, t:t + 1], kq), AF.Sqrt)
            O.recip((rstd[:, t:t + 1], kq), (rstd[:, t:t + 1], kq))
            O.stt('dve', sh_[i][:], xm[i][:], (rstd[:, t:t + 1], kq), gfin[:], ALU.mult, ALU.mult)
            fin.append(O.dma('sp', out[rows, :], sh_[i][:]))
        need = {}
        for k, v in fin:
            if need.get(k, 0) < v:
                need[k] = v
        Sd._waits('sp', need)
        Sd.barrier()


def build_fused(cfg=None):
    cfg = cfg or {}
    n_exp = cfg.get('n_exp', NEXP_LOCAL)
    specs = dict(S1_SPECS)
    specs.update(S2_SPECS)
    for k in ('weg', 'weu', 'wed'):
        specs[k] = ([n_exp] + specs[k][0][1:], F32)
    C = Ctx(specs)
    nc = C.nc
    scr = {
        'agh_in': nc.dram_tensor('agh_in', [D, 1024], BF16), 'agh_out': nc.dram_tensor('agh_out', [NCORES * D, 1024], BF16),
        'agw_in': nc.dram_tensor('agw_in', [1024, 256], F32), 'agw_out': nc.dram_tensor('agw_out', [8192, 256], F32),
        'rs2_in': nc.dram_tensor('rs2_in', [8192, D], F32), 'rs2_out': nc.dram_tensor('rs2_out', [1024, D], F32),
        'shr_d': nc.dram_tensor('shr_d', [1024, D], F32), 'xmid_d': nc.dram_tensor('xmid_d', [1024, D], F32),
        'modT_d': nc.dram_tensor('modT_d', [96, 128], F32),
        'wgb': nc.dram_tensor('wgb', [n_exp, D, 512], BF16), 'wub': nc.dram_tensor('wub', [n_exp, D, 512], BF16),
        'wdb': nc.dram_tensor('wdb', [n_exp, 512, D], BF16),
        'wsgb': nc.dram_tensor('wsgb', [1, D, 512], BF16), 'wsub': nc.dram_tensor('wsub', [1, D, 512], BF16),
        'wsdb': nc.dram_tensor('wsdb', [1, 512, D], BF16),
    }
    ex = {'h2T': scr['agh_in'].ap(), 'gw': scr['agw_in'].ap(), 'xmid': scr['xmid_d'].ap(), 'modT': scr['modT_d'].ap(),
          'out': C.OUT('out', [1024, D])}
    with ExitStack() as es:
        Sd = Sched(nc, es)
        O = Ops(Sd)
        ps = [es.enter_context(nc.psum_tensor('ps%d' % i, [128, 512], F32)) for i in range(8)]
        emit_convert(C, Sd, O, scr, n_exp)
        emit_stage1(C, Sd, O, es, ps, cfg, ex)
        emit_stage2(C, Sd, O, es, ps, cfg, ex, scr)
        print("fused instructions:", Sd.n_inst, flush=True)
    return C


def prep_stage2(inp, maps):
    f = lambda k: np.asarray(inp[k])
    gfin = np.ascontiguousarray(np.broadcast_to(f('g_final')[None, :], (128, D))).astype(np.float32)
    for r in range(NCORES):
        m = maps[r]
        goh = np.zeros((128, NCORES), np.float32); goh[:, r] = 1.0
        m['goh'] = goh
        m['gfin'] = gfin
        m['weg'] = f('w_exp_gate')[0][NEXP_LOCAL * r:NEXP_LOCAL * (r + 1)]
        m['weu'] = f('w_exp_up')[0][NEXP_LOCAL * r:NEXP_LOCAL * (r + 1)]
        m['wed'] = f('w_exp_down')[0][NEXP_LOCAL * r:NEXP_LOCAL * (r + 1)]
        m['wsg'] = f('w_sh_gate'); m['wsu'] = f('w_sh_up'); m['wsd'] = f('w_sh_down')
    return maps


_CACHE = {}


def kernel_fused(**inputs):
    if 'prog' not in _CACHE:
        _CACHE['prog'] = build_fused({})
    C = _CACHE['prog']
    maps = prep_stage1(inputs)
    maps = prep_stage2(inputs, maps)
    in_maps = [{k: np.ascontiguousarray(m[k]) for k in C.in_names} for m in maps]
    res = run_bass_kernel_spmd(C.nc, in_maps, core_ids=list(range(NCORES)))
    out = np.zeros((NB, S, D), np.float32)
    for r in range(NCORES):
        b, g = r // 2, r % 2
        out[b, 1024 * g:1024 * (g + 1), :] = np.asarray(res.results[r]['out'])
    return out


class _W:
    def __init__(self, ap):
        self._ap = ap

    def ap(self):
        return self._ap


def build_moe_multi(cfg=None):
    cfg = dict(cfg or {}); cfg['multi'] = True
    n_exp = cfg.get('n_exp', NEXP_LOCAL)
    specs = dict(S2_SPECS)
    specs.update({'h2all': ([NCORES * D, 1024], BF16), 'gwall': ([8192, 256], F32), 'h2own': ([D, 1024], BF16)})
    C = Ctx(specs)
    nc = C.nc
    scr = {
        'agh_in': _W(C.IN('h2own')), 'agh_out': _W(C.IN('h2all')), 'agw_out': _W(C.IN('gwall')),
        'rs2_in': _W(C.OUT('partial', [8192, D])), 'shr_d': _W(C.OUT('shr', [1024, D])),
        'wgb': nc.dram_tensor('wgb', [n_exp, D, 512], BF16), 'wub': nc.dram_tensor('wub', [n_exp, D, 512], BF16),
        'wdb': nc.dram_tensor('wdb', [n_exp, 512, D], BF16),
        'wsgb': nc.dram_tensor('wsgb', [1, D, 512], BF16), 'wsub': nc.dram_tensor('wsub', [1, D, 512], BF16),
        'wsdb': nc.dram_tensor('wsdb', [1, 512, D], BF16),
    }
    with ExitStack() as es:
        Sd = Sched(nc, es)
        O = Ops(Sd)
        ps = [es.enter_context(nc.psum_tensor('ps%d' % i, [128, 512], F32)) for i in range(8)]
        emit_convert(C, Sd, O, scr, n_exp)
        emit_stage2(C, Sd, O, es, ps, cfg, {}, scr)
    return C


def build_final_multi(cfg=None):
    specs = {'parts': ([NCORES, 1024, D], F32), 'shr': ([1024, D], F32), 'xmid': ([1024, D], F32),
             'modT': ([96, 128], F32), 'gfin': ([128, D], F32)}
    C = Ctx(specs)
    nc = C.nc
    ex = {'xmid': C.IN('xmid'), 'modT': C.IN('modT'), 'out': C.OUT('out', [1024, D])}
    scr = {'shr_d': _W(C.IN('shr'))}
    with ExitStack() as es:
        Sd = Sched(nc, es)
        O = Ops(Sd)
        emit_final(C, Sd, O, es, cfg or {}, ex, scr, C.IN('parts'))
    return C


def kernel_multi(**inputs):
    if 'm1' not in _CACHE:
        _CACHE['m1'] = build_stage1({})
        _CACHE['m2'] = build_moe_multi({})
        _CACHE['m3'] = build_final_multi({})
    C1, C2, C3 = _CACHE['m1'], _CACHE['m2'], _CACHE['m3']
    cores = list(range(NCORES))
    maps = prep_stage1(inputs)
    r1 = run_bass_kernel_spmd(C1.nc, [{k: np.ascontiguousarray(m[k]) for k in C1.in_names} for m in maps], core_ids=cores).results
    h2all = np.concatenate([np.asarray(r1[r]['h2T']) for r in cores], axis=0)
    gwall = np.concatenate([np.asarray(r1[r]['gw']) for r in cores], axis=0)
    m2 = prep_stage2(inputs, [dict() for _ in cores])
    for r in cores:
        m2[r]['h2all'] = h2all; m2[r]['gwall'] = gwall; m2[r]['h2own'] = np.asarray(r1[r]['h2T'])
    r2 = run_bass_kernel_spmd(C2.nc, [{k: np.ascontiguousarray(m[k]) for k in C2.in_names} for m in m2], core_ids=cores).results
    m3 = []
    for r in cores:
        parts = np.stack([np.asarray(r2[q]['partial'])[1024 * r:1024 * (r + 1)] for q in cores], axis=0)
        m3.append({'parts': parts, 'shr': np.asarray(r2[r]['shr']), 'xmid': np.asarray(r1[r]['xmid']),
                   'modT': np.asarray(r1[r]['modT']), 'gfin': m2[r]['gfin']})
    r3 = run_bass_kernel_spmd(C3.nc, [{k: np.ascontiguousarray(m[k]) for k in C3.in_names} for m in m3], core_ids=cores).results
    out = np.zeros((NB, S, D), np.float32)
    for r in cores:
        b, g = r // 2, r % 2
        out[b, 1024 * g:1024 * (g + 1), :] = np.asarray(r3[r]['out'])
    return out


def kernel(**inputs):
    return kernel_multi(**inputs)
```
